# Optimizing a Trainium2 kernel written in Bass

```python
import math
import jax, jax.numpy as jnp
from jax import lax
import numpy as np


D_MODEL = 4096
BATCH = 1
SEQ = 16384
DEPTH = 4

HEAD_DIM = 128
DN_HEADS = 16
DN_DK = 128
DN_DV = 128
DN_QK_W = DN_HEADS * DN_DK
DN_V_W = DN_HEADS * DN_DV
DN_CONV_W = 2 * DN_QK_W + DN_V_W
CONV_K = 4
CHUNK = 64
SWA_Q_HEADS = 16
SWA_KV_HEADS = 4
SWA_GROUP = SWA_Q_HEADS // SWA_KV_HEADS
SWA_Q_W = SWA_Q_HEADS * HEAD_DIM
SWA_KV_W = SWA_KV_HEADS * HEAD_DIM
WINDOW = 128
BLOCK = 128
MIX_WIDTH = DN_V_W + SWA_Q_W
SPLITS = (DN_CONV_W, DN_V_W, DN_HEADS, DN_HEADS, SWA_Q_W, SWA_KV_W, SWA_KV_W)
N_IN = sum(SPLITS)
D_FF = 8192
N_EXPERTS = 8
TOP_K = 2
D_FF_EXPERT = 2048
N_DENSE = (DEPTH + 1) // 2
N_MOE = DEPTH // 2
DN_ALPHA = (2 * DEPTH) ** 0.25
DN_BETA = (8 * DEPTH) ** -0.25
LN_EPS = 1e-5
NEG_INF = -1e30

kernel_name = 'hybrid_deltanet_swa_sink_alibi_deepnorm_moe'


def layer_norm(x, g, b):
    xf = x.astype(jnp.float32)
    mu = jnp.mean(xf, -1, keepdims=True)
    var = jnp.mean(jnp.square(xf - mu), -1, keepdims=True)
    return ((xf - mu) * lax.rsqrt(var + LN_EPS) * g + b).astype(x.dtype)


def l2norm(t):
    return t * lax.rsqrt(jnp.sum(jnp.square(t), -1, keepdims=True) + 1e-6)


def causal_short_conv(u, w):
    c = u.shape[-1]
    return lax.conv_general_dilated(u, w[:, None, :].astype(u.dtype), window_strides=(1,),
                                    padding=[(CONV_K - 1, 0)],
                                    dimension_numbers=('NWC', 'WIO', 'NWC'),
                                    feature_group_count=c)


def gated_delta_rule(q, k, v, g, beta):
    f32 = jnp.float32
    b_, t_, h_, dk = q.shape
    dv = v.shape[-1]
    n = t_ // CHUNK

    def chunks(a):
        return a.astype(f32).reshape(b_, n, CHUNK, h_, -1).transpose(0, 1, 3, 2, 4)

    qc = chunks(q) * (dk ** -0.5)
    kc = chunks(k)
    vc = chunks(v)
    gc = g.astype(f32).reshape(b_, n, CHUNK, h_).transpose(0, 1, 3, 2)
    bc = beta.astype(f32).reshape(b_, n, CHUNK, h_).transpose(0, 1, 3, 2)
    gcum = jnp.cumsum(gc, -1)
    causal = jnp.tril(jnp.ones((CHUNK, CHUNK), bool))
    strict = jnp.tril(jnp.ones((CHUNK, CHUNK), bool), k=-1)
    diff = gcum[..., :, None] - gcum[..., None, :]
    decay = jnp.exp(jnp.where(causal, diff, NEG_INF))
    kk = jnp.einsum('bnhid,bnhjd->bnhij', kc, kc)
    lower = jnp.eye(CHUNK, dtype=f32) + jnp.where(strict, bc[..., :, None] * kk * decay, 0.0)
    rhs = jnp.concatenate([vc * bc[..., None], kc * (bc * jnp.exp(gcum))[..., None]], -1)
    sol = lax.linalg.triangular_solve(lower, rhs, left_side=True, lower=True, unit_diagonal=True)
    u = sol[..., :dv]
    w = sol[..., dv:]
    a_qk = jnp.einsum('bnhid,bnhjd->bnhij', qc, kc) * decay
    q_dec = qc * jnp.exp(gcum)[..., None]
    k_dec = kc * jnp.exp(gcum[..., -1:] - gcum)[..., None]
    g_last = jnp.exp(gcum[..., -1])

    def step(s, inp):
        u_i, w_i, a_i, qd_i, kd_i, gl_i = inp
        v_new = u_i - jnp.einsum('bhcd,bhde->bhce', w_i, s)
        o = jnp.einsum('bhcd,bhde->bhce', qd_i, s) + jnp.einsum('bhij,bhje->bhie', a_i, v_new)
        s = s * gl_i[..., None, None] + jnp.einsum('bhcd,bhce->bhde', kd_i, v_new)
        return s, o

    xs = (u.swapaxes(0, 1), w.swapaxes(0, 1), a_qk.swapaxes(0, 1),
          q_dec.swapaxes(0, 1), k_dec.swapaxes(0, 1), g_last.swapaxes(0, 1))
    s0 = jnp.zeros((b_, h_, dk, dv), f32)
    _, o = lax.scan(step, s0, xs)
    return o.transpose(1, 0, 3, 2, 4).reshape(b_, t_, h_, dv)


def alibi_slopes(n):
    return jnp.exp2(-8.0 * jnp.arange(1, n + 1, dtype=jnp.float32) / n)


def sliding_window_sink_attention(q, k, v, sinks):
    b_, t_, _ = q.shape
    nb = t_ // BLOCK
    qb = q.reshape(b_, nb, BLOCK, SWA_KV_HEADS, SWA_GROUP, HEAD_DIM)

    def with_prev(a):
        a = a.reshape(b_, nb, BLOCK, SWA_KV_HEADS, HEAD_DIM)
        prev = jnp.pad(a, ((0, 0), (1, 0), (0, 0), (0, 0), (0, 0)))[:, :-1]
        return jnp.concatenate([prev, a], axis=2)

    kw = with_prev(k)
    vw = with_prev(v)
    s = jnp.einsum('bnqhgd,bnkhd->bnhgqk', qb, kw).astype(jnp.float32) * (HEAD_DIM ** -0.5)
    qi = jnp.arange(BLOCK)[:, None]
    kj = jnp.arange(2 * BLOCK)[None, :]
    dist = qi - kj + BLOCK
    in_window = (dist >= 0) & (dist < WINDOW)
    blk = jnp.arange(nb)[:, None, None]
    valid = in_window[None] & ((blk > 0) | (kj[None] >= BLOCK))
    slopes = alibi_slopes(SWA_Q_HEADS).reshape(SWA_KV_HEADS, SWA_GROUP)
    logits = s - slopes[:, :, None, None] * dist.astype(jnp.float32)
    logits = jnp.where(valid[None, :, None, None], logits, NEG_INF)
    sink = jnp.broadcast_to(sinks.astype(jnp.float32).reshape(SWA_KV_HEADS, SWA_GROUP)[:, :, None, None],
                            logits.shape[:-1] + (1,))
    probs = jax.nn.softmax(jnp.concatenate([logits, sink], -1), -1)[..., :-1]
    o = jnp.einsum('bnhgqk,bnkhd->bnqhgd', probs.astype(v.dtype), vw)
    return o.reshape(b_, t_, SWA_Q_W)


def hybrid_mixer(h, w_in, conv_w, a_log, dt_bias, dn_norm_w, sinks, w_o):
    b_, t_, _ = h.shape
    f32 = jnp.float32
    proj = jnp.einsum('btd,dn->btn', h, w_in)
    idx = np.cumsum(SPLITS)[:-1].tolist()
    dn_qkv, dn_z, dn_b, dn_a, sw_q, sw_k, sw_v = jnp.split(proj, idx, axis=-1)
    qkv = jax.nn.silu(causal_short_conv(dn_qkv, conv_w))
    q, k, v = jnp.split(qkv, [DN_QK_W, 2 * DN_QK_W], axis=-1)
    q = l2norm(q.reshape(b_, t_, DN_HEADS, DN_DK).astype(f32))
    k = l2norm(k.reshape(b_, t_, DN_HEADS, DN_DK).astype(f32))
    v = v.reshape(b_, t_, DN_HEADS, DN_DV)
    beta = jax.nn.sigmoid(dn_b.astype(f32))
    g = -jnp.exp(a_log.astype(f32)) * jax.nn.softplus(dn_a.astype(f32) + dt_bias.astype(f32))
    o = gated_delta_rule(q, k, v, g, beta)
    o = o * lax.rsqrt(jnp.mean(jnp.square(o), -1, keepdims=True) + 1e-6) * dn_norm_w.astype(f32)
    o = o * jax.nn.silu(dn_z.astype(f32).reshape(b_, t_, DN_HEADS, DN_DV))
    out_a = o.reshape(b_, t_, DN_V_W).astype(h.dtype)
    out_b = sliding_window_sink_attention(sw_q, sw_k, sw_v, sinks)
    return jnp.einsum('btm,md->btd', jnp.concatenate([out_a, out_b], -1), w_o)


def swiglu(x, w1, w3, w2):
    return jnp.einsum('btf,fd->btd', jax.nn.silu(jnp.einsum('btd,df->btf', x, w1)) * jnp.einsum('btd,df->btf', x, w3), w2)


def moe_ffn(x, router_w, w1, w3, w2):
    logits = jnp.einsum('btd,de->bte', x, router_w).astype(jnp.float32)
    top_v, top_i = lax.top_k(logits, TOP_K)
    top_w = jax.nn.softmax(top_v, -1)
    gates = jnp.sum(jax.nn.one_hot(top_i, N_EXPERTS, dtype=jnp.float32) * top_w[..., None], axis=-2)
    out = jnp.zeros_like(x)
    for e in range(N_EXPERTS):
        out = out + gates[..., e:e + 1].astype(x.dtype) * swiglu(x, w1[e], w3[e], w2[e])
    return out


def setup_inputs(seed: int = 0) -> dict:
    key = jax.random.key(seed)
    ks = jax.random.split(key, 24)
    f32 = jnp.float32
    nrm = lambda k, shape, scale: jax.random.normal(k, shape, f32) * scale
    dt = jnp.exp(jax.random.uniform(ks[4], (DEPTH, DN_HEADS), f32, minval=math.log(1e-3), maxval=math.log(1e-1)))
    return {
        'x': nrm(ks[0], (BATCH, SEQ, D_MODEL), 1.0),
        'w_in': nrm(ks[1], (DEPTH, D_MODEL, N_IN), D_MODEL ** -0.5),
        'conv_w': nrm(ks[2], (DEPTH, CONV_K, DN_CONV_W), CONV_K ** -0.5),
        'a_log': jnp.log(jax.random.uniform(ks[3], (DEPTH, DN_HEADS), f32, minval=1.0, maxval=16.0)),
        'dt_bias': dt + jnp.log(-jnp.expm1(-dt)),
        'dn_norm_w': 1.0 + nrm(ks[5], (DEPTH, DN_DV), 0.02),
        'sinks': nrm(ks[6], (DEPTH, SWA_Q_HEADS), 1.0),
        'w_o': nrm(ks[7], (DEPTH, MIX_WIDTH, D_MODEL), DN_BETA * MIX_WIDTH ** -0.5),
        'ln1_g': 1.0 + nrm(ks[8], (DEPTH, D_MODEL), 0.02),
        'ln1_b': nrm(ks[9], (DEPTH, D_MODEL), 0.02),
        'ffn_w1': nrm(ks[10], (N_DENSE, D_MODEL, D_FF), D_MODEL ** -0.5),
        'ffn_w3': nrm(ks[11], (N_DENSE, D_MODEL, D_FF), D_MODEL ** -0.5),
        'ffn_w2': nrm(ks[12], (N_DENSE, D_FF, D_MODEL), DN_BETA * D_FF ** -0.5),
        'router_w': nrm(ks[13], (N_MOE, D_MODEL, N_EXPERTS), D_MODEL ** -0.5),
        'exp_w1': nrm(ks[14], (N_MOE, N_EXPERTS, D_MODEL, D_FF_EXPERT), D_MODEL ** -0.5),
        'exp_w3': nrm(ks[15], (N_MOE, N_EXPERTS, D_MODEL, D_FF_EXPERT), D_MODEL ** -0.5),
        'exp_w2': nrm(ks[16], (N_MOE, N_EXPERTS, D_FF_EXPERT, D_MODEL), DN_BETA * D_FF_EXPERT ** -0.5),
        'ln2_g': 1.0 + nrm(ks[17], (DEPTH, D_MODEL), 0.02),
        'ln2_b': nrm(ks[18], (DEPTH, D_MODEL), 0.02),
    }


def reference(x, w_in, conv_w, a_log, dt_bias, dn_norm_w, sinks, w_o, ln1_g, ln1_b,
              ffn_w1, ffn_w3, ffn_w2, router_w, exp_w1, exp_w3, exp_w2, ln2_g, ln2_b):
    for i in range(DEPTH):
        mix = hybrid_mixer(x, w_in[i], conv_w[i], a_log[i], dt_bias[i], dn_norm_w[i], sinks[i], w_o[i])
        x = layer_norm(DN_ALPHA * x + mix, ln1_g[i], ln1_b[i])
        if i % 2 == 0:
            j = i // 2
            f = swiglu(x, ffn_w1[j], ffn_w3[j], ffn_w2[j])
        else:
            j = i // 2
            f = moe_ffn(x, router_w[j], exp_w1[j], exp_w3[j], exp_w2[j])
        x = layer_norm(DN_ALPHA * x + f, ln2_g[i], ln2_b[i])
    return x
```

```python
import numpy as np
from contextlib import ExitStack
import concourse.bass as bass
import concourse.mybir as mybir
from concourse.bass_utils import run_bass_kernel_spmd

F32 = mybir.dt.float32
BF16 = mybir.dt.bfloat16
AF = mybir.ActivationFunctionType
ALU = mybir.AluOpType
AX = mybir.AxisListType

D_MODEL = 4096
SEQ = 16384
DEPTH = 4
NCORE = 8
KC = D_MODEL // 128
DN_ALPHA = (2 * DEPTH) ** 0.25
LN_EPS = 1e-5
NEG = -1e30
QSCALE = 128 ** -0.5


class Res:
    __slots__ = ("name", "lw", "rd", "sem", "cnt")

    def __init__(self, name):
        self.name = name
        self.lw = None
        self.rd = []
        self.sem = None
        self.cnt = 0


class V:
    __slots__ = ("ap", "res")

    def __init__(self, ap, res):
        self.ap = ap
        self.res = res


class Buf:
    def __init__(self, name, t, nres=1):
        self.name = name
        self.t = t
        self.res = [Res(f"{name}.{i}") for i in range(nres)]

    def v(self, idx=None, r=None):
        ap = self.t[:] if idx is None else self.t[idx]
        if r is None:
            rs = self.res
        elif isinstance(r, int):
            rs = [self.res[r]]
        else:
            rs = [self.res[i] for i in r]
        return V(ap, rs)


class Op:
    __slots__ = ("id", "eng", "fn", "deps", "dma", "semres", "tok", "inc", "ms")


EPOCH = 20000


class TR:
    ENG = ("pe", "act", "dve", "pool", "sp")

    def __init__(self, nc, es):
        self.nc = nc
        self.es = es
        self.ops = []
        self.nbuf = 0

    def sbuf(self, name, shape, dt, nres=1):
        t = self.es.enter_context(self.nc.sbuf_tensor(name, list(shape), dt))
        return Buf(name, t, nres)

    def psum(self, name, shape, dt):
        t = self.es.enter_context(self.nc.psum_tensor(name, list(shape), dt))
        return Buf(name, t, 1)

    def dram(self, name, shape, dt, kind):
        t = self.nc.dram_tensor(name, list(shape), dt, kind=kind).ap()
        return Buf(name, t, 1)

    def op(self, eng, fn, reads=(), writes=(), dma=False, semres=None):
        o = Op()
        o.id = len(self.ops)
        o.eng = eng
        o.fn = fn
        o.dma = dma
        o.semres = semres
        o.inc = False
        o.ms = None
        o.tok = None
        deps = {}
        rres = []
        for v in reads:
            rres.extend(v.res)
        wres = []
        for v in writes:
            wres.extend(v.res)
        for r in rres:
            if r.lw is not None:
                deps[r.lw] = "raw"
        for w in wres:
            if w.lw is not None and w.lw not in deps:
                deps[w.lw] = "waw"
            for x in w.rd:
                if x not in deps:
                    deps[x] = "war"
        for r in rres:
            r.rd.append(o.id)
        for w in wres:
            w.lw = o.id
            w.rd = []
        keep = []
        for d, kind in deps.items():
            p = self.ops[d]
            if (not p.dma) and (not dma) and p.eng == eng:
                if eng == "pe":
                    continue
                if kind != "raw":
                    continue
            keep.append(d)
        o.deps = sorted(keep)
        self.ops.append(o)
        return o

    def dma(self, q, out, in_, semside):
        o_ap, i_ap = out.ap, in_.ap
        return self.op(q, lambda e, s, n: e.dma_start(out=o_ap, in_=i_ap).then_inc(s, 16),
                       reads=[in_], writes=[out], dma=True, semres=semside.res[0])

    def emit(self):
        nc = self.nc
        ops = self.ops
        for o in ops:
            if o.dma:
                r = o.semres
                if r.sem is None:
                    r.sem = self.es.enter_context(nc.semaphore(f"d{o.id}"))
                r.cnt += 16
                o.tok = (r.sem, r.cnt)
        waited = {e: {} for e in self.ENG}
        for o in ops:
            w = waited[o.eng]
            for d in o.deps:
                p = ops[d]
                if p.dma:
                    continue
                if w.get(p.eng, -1) < p.id:
                    p.inc = True
                    w[p.eng] = p.id
        cnt = {e: 0 for e in self.ENG}
        esems = {}
        for o in ops:
            if (not o.dma) and o.inc:
                k = cnt[o.eng]
                cnt[o.eng] += 1
                ep = k // EPOCH
                key = (o.eng, ep)
                if key not in esems:
                    esems[key] = self.es.enter_context(nc.semaphore(f"e_{o.eng}_{ep}"))
                o.tok = (esems[key], k % EPOCH + 1)
        streams = {e: [] for e in self.ENG}
        waited = {e: {} for e in self.ENG}
        for o in ops:
            w = waited[o.eng]
            st = streams[o.eng]
            for d in o.deps:
                p = ops[d]
                if p.tok is None:
                    continue
                sem, val = p.tok
                key = id(sem)
                if w.get(key, 0) < val:
                    w[key] = val
                    st.append(("w", sem, val))
            st.append(("o", o))
        seen = {}
        for o in ops:
            if o.dma:
                seen[id(o.semres.sem)] = (o.semres.sem, o.semres.cnt)
        for sem, val in seen.values():
            streams["sp"].append(("w", sem, val))

        def run(eng, st):
            for it in st:
                if it[0] == "w":
                    eng.wait_ge(it[1], it[2])
                else:
                    o = it[1]
                    if o.dma:
                        o.fn(eng, o.tok[0], 16)
                    else:
                        ins = o.fn(eng)
                        if o.inc:
                            ins.then_inc(o.tok[0], 1)

        with nc.Block() as block:
            @block.tensor
            def _(e):
                run(e, streams["pe"])

            @block.scalar
            def _(e):
                run(e, streams["act"])

            @block.vector
            def _(e):
                run(e, streams["dve"])

            @block.gpsimd
            def _(e):
                run(e, streams["pool"])

            @block.sync
            def _(e):
                run(e, streams["sp"])

    def mm(self, out, lhsT, rhs, start=True, stop=True):
        o, l, r = out.ap, lhsT.ap, rhs.ap
        return self.op("pe", lambda e: e.matmul(o, l, r, start=start, stop=stop),
                       reads=[lhsT, rhs], writes=[out])

    def tp(self, out, in_, ident):
        o, i, d = out.ap, in_.ap, ident.ap
        return self.op("pe", lambda e: e.transpose(o, i, d), reads=[in_, ident], writes=[out])

    def act(self, out, in_, func, bias=None, scale=None, accum=None, eng="act"):
        o, i = out.ap, in_.ap
        kw = {}
        reads = [in_]
        writes = [out]
        if bias is not None:
            if isinstance(bias, V):
                kw["bias"] = bias.ap
                reads.append(bias)
            else:
                kw["bias"] = bias
        if scale is not None:
            if isinstance(scale, V):
                kw["scale"] = scale.ap
                reads.append(scale)
            else:
                kw["scale"] = scale
        if accum is not None:
            kw["accum_out"] = accum.ap
            writes.append(accum)
        return self.op(eng, lambda e: e.activation(o, i, func, **kw), reads=reads, writes=writes)

    def ts(self, out, in0, s1, s2, op0, op1=None, eng="dve"):
        o, i = out.ap, in0.ap
        reads = [in0]
        a1 = s1
        a2 = s2
        if isinstance(s1, V):
            a1 = s1.ap
            reads.append(s1)
        if isinstance(s2, V):
            a2 = s2.ap
            reads.append(s2)
        if op1 is None:
            return self.op(eng, lambda e: e.tensor_scalar(o, i, a1, None, op0), reads=reads, writes=[out])
        return self.op(eng, lambda e: e.tensor_scalar(o, i, a1, a2, op0, op1), reads=reads, writes=[out])

    def stt(self, out, in0, s, in1, op0, op1, eng="dve"):
        o, i0, i1 = out.ap, in0.ap, in1.ap
        reads = [in0, in1]
        a = s
        if isinstance(s, V):
            a = s.ap
            reads.append(s)
        return self.op(eng, lambda e: e.scalar_tensor_tensor(o, i0, a, i1, op0, op1), reads=reads, writes=[out])

    def tt(self, out, in0, in1, op, eng="dve"):
        o, i0, i1 = out.ap, in0.ap, in1.ap
        return self.op(eng, lambda e: e.tensor_tensor(o, i0, i1, op), reads=[in0, in1], writes=[out])

    def cp(self, out, in_, eng="dve"):
        o, i = out.ap, in_.ap
        if eng == "act":
            return self.op(eng, lambda e: e.copy(o, i), reads=[in_], writes=[out])
        return self.op(eng, lambda e: e.tensor_copy(o, i), reads=[in_], writes=[out])

    def memset(self, out, val, eng="dve"):
        o = out.ap
        return self.op(eng, lambda e: e.memset(o, val), reads=[], writes=[out])

    def rmax(self, out, in_):
        o, i = out.ap, in_.ap
        return self.op("dve", lambda e: e.tensor_reduce(o, i, AX.X, ALU.max), reads=[in_], writes=[out])


class Rot:
    def __init__(self, bufs):
        self.bufs = bufs
        self.i = 0

    def next(self):
        b = self.bufs[self.i % len(self.bufs)]
        self.i += 1
        return b


def emit_ln_fm(t, X, N, ps, ones, lnp, gi, bi, sqrot, meanB, rstdB, tmpB, epsv, post=None):
    S = ps.next()
    Q = ps.next()
    for kc in range(KC):
        t.mm(S.v((slice(None), slice(0, N))), ones.v(), X.v((slice(None), kc, slice(None)), kc),
             start=(kc == 0), stop=(kc == KC - 1))
    for kc in range(KC):
        sq = sqrot.next()
        t.act(sq.v(), X.v((slice(None), kc, slice(None)), kc), AF.Square)
        t.mm(Q.v((slice(None), slice(0, N))), ones.v(), sq.v(), start=(kc == 0), stop=(kc == KC - 1))
    t.cp(meanB.v(), S.v((slice(None), slice(0, N))), eng="act")
    t.tt(tmpB.v(), meanB.v(), meanB.v(), ALU.mult)
    t.tt(tmpB.v(), Q.v((slice(None), slice(0, N))), tmpB.v(), ALU.subtract)
    t.act(rstdB.v(), tmpB.v(), AF.Ln, bias=epsv, scale=1.0)
    t.act(rstdB.v(), rstdB.v(), AF.Exp, scale=-0.5)
    for kc in range(KC):
        xv = X.v((slice(None), kc, slice(None)), kc)
        t.tt(xv, xv, meanB.v(), ALU.subtract)
        t.tt(xv, xv, rstdB.v(), ALU.mult)
        t.ts(xv, xv, lnp.v((slice(None), gi, slice(kc, kc + 1))), lnp.v((slice(None), bi, slice(kc, kc + 1))),
             ALU.mult, ALU.add)
        if post is not None:
            post(kc, xv)


def build_B(T, moe):
    TG = 512
    NE = 8 if moe else 4
    nc = bass.Bass("TRN2", target_bir_lowering=False)
    es = ExitStack()
    with es:
        t = TR(nc, es)
        xT = t.dram("xT", [D_MODEL, T], F32, "ExternalInput")
        mixT = t.dram("mixT", [D_MODEL, T], F32, "ExternalInput")
        w_o = t.dram("w_o", [D_MODEL, D_MODEL], F32, "ExternalInput")
        lnp_d = t.dram("lnp", [128, 4, KC], F32, "ExternalInput")
        ones_d = t.dram("onesd", [128, 128], F32, "ExternalInput")
        ident_d = t.dram("ident", [128, 128], F32, "ExternalInput")
        w1 = t.dram("w1", [NE, D_MODEL, 2048], F32, "ExternalInput")
        w3 = t.dram("w3", [NE, D_MODEL, 2048], F32, "ExternalInput")
        w2 = t.dram("w2", [NE, 2048, D_MODEL], F32, "ExternalInput")
        if moe:
            rw_d = t.dram("rw", [128, KC, 8], F32, "ExternalInput")
        yT = t.dram("yT", [D_MODEL, T], F32, "ExternalOutput")

        X = t.sbuf("X", [128, KC, TG], F32, nres=KC)
        A = t.sbuf("A", [128, KC, TG], BF16, nres=KC)
        WS = Rot([t.sbuf(f"WS{i}", [128, 8192], BF16) for i in range(3 if moe else 4)])
        H = t.sbuf("H", [128, 16, TG], BF16, nres=16)
        lnp = t.sbuf("lnp_s", [128, 4, KC], F32)
        ones = t.sbuf("ones_s", [128, 128], F32)
        ident = t.sbuf("ident_s", [128, 128], F32)
        sqrot = Rot([t.sbuf(f"sq{i}", [128, TG], F32) for i in range(2)])
        meanB = t.sbuf("meanB", [128, TG], F32)
        rstdB = t.sbuf("rstdB", [128, TG], F32)
        tmpB = t.sbuf("tmpB", [128, TG], F32)
        sgrot = Rot([t.sbuf(f"sg{i}", [128, TG], F32) for i in range(2)])
        ps = Rot([t.psum(f"ps{i}", [128, 512], F32) for i in range(8)])
        if moe:
            RW = t.sbuf("RW", [128, KC, 8], F32)
            GATE = t.sbuf("GATE", [128, 8, TG], F32, nres=8)
            LG = t.sbuf("LG", [128, 8], F32)
            L2 = t.sbuf("L2", [128, 8], F32)
            EQ = t.sbuf("EQ", [128, 8], F32)
            GT = t.sbuf("GT", [128, 8], F32)
            sm = t.sbuf("sm", [128, 8], F32, nres=8)
            REP = Rot([t.sbuf(f"rep{i}", [128, 128], F32) for i in range(2)])
            onesfull = t.sbuf("onesfull", [128, 128], F32)
            t.dma("sp", RW.v(), rw_d.v(), RW.v())
            t.memset(onesfull.v(), 1.0)

        epsb = t.sbuf("epsb", [128, 1], F32)
        t.memset(epsb.v(), LN_EPS)
        t.dma("sp", lnp.v(), lnp_d.v(), lnp.v())
        t.dma("sp", ones.v(), ones_d.v(), ones.v())
        t.dma("sp", ident.v(), ident_d.v(), ident.v())

        xT3 = xT.t.rearrange("(kc p) t -> p kc t", p=128)
        mixT3 = mixT.t.rearrange("(kc p) t -> p kc t", p=128)
        yT3 = yT.t.rearrange("(kc p) t -> p kc t", p=128)
        wo3 = w_o.t.rearrange("(kc p) n -> p kc n", p=128)

        for g in range(T // TG):
            t0 = g * TG
            for half in range(2):
                ks = slice(half * 16, half * 16 + 16)
                t.dma("sp", V(X.t[:, ks, :], X.res[half * 16:half * 16 + 16]),
                      V(xT3[:, ks, t0:t0 + TG], xT.res), V(X.t[:, ks, :], [X.res[half * 16]]))
                t.dma("pool", V(A.t[:, ks, :], A.res[half * 16:half * 16 + 16]),
                      V(mixT3[:, ks, t0:t0 + TG], mixT.res), V(A.t[:, ks, :], [A.res[half * 16]]))
            for ds in range(D_MODEL // 256):
                W = WS.next()
                Wv = W.t[:, :].rearrange("p (kc n) -> p kc n", kc=KC)
                t.dma("pool", V(Wv, W.res), V(wo3[:, :, ds * 256:(ds + 1) * 256], w_o.res), W.v())
                for j in range(2):
                    dt_ = ds * 2 + j
                    P = ps.next()
                    for kc in range(KC):
                        t.mm(P.v(), V(Wv[:, kc, j * 128:(j + 1) * 128], W.res),
                             A.v((slice(None), kc, slice(None)), kc), start=(kc == 0), stop=(kc == KC - 1))
                    xv = X.v((slice(None), dt_, slice(None)), dt_)
                    t.stt(xv, xv, DN_ALPHA, P.v(), ALU.mult, ALU.add)
            def post1(kc, xv):
                t.cp(A.v((slice(None), kc, slice(None)), kc), xv, eng="act")
            emit_ln_fm(t, X, TG, ps, ones, lnp, 0, 1, sqrot, meanB, rstdB, tmpB, epsb.v(), post=post1)
            if moe:
                for j in range(TG // 128):
                    R = ps.next()
                    for kc in range(KC):
                        t.mm(R.v((slice(None), slice(0, 8))), X.v((slice(None), kc, slice(j * 128, (j + 1) * 128)), kc),
                             RW.v((slice(None), kc, slice(None))), start=(kc == 0), stop=(kc == KC - 1))
                    t.cp(LG.v(), R.v((slice(None), slice(0, 8))))
                    m1 = sm.v((slice(None), slice(0, 1)), 0)
                    m2 = sm.v((slice(None), slice(1, 2)), 1)
                    nm1 = sm.v((slice(None), slice(2, 3)), 2)
                    ssum = sm.v((slice(None), slice(3, 4)), 3)
                    rs = sm.v((slice(None), slice(4, 5)), 4)
                    t.rmax(m1, LG.v())
                    t.ts(EQ.v(), LG.v(), m1, NEG, ALU.is_equal, ALU.mult)
                    t.tt(L2.v(), LG.v(), EQ.v(), ALU.add)
                    t.rmax(m2, L2.v())
                    t.ts(EQ.v(), LG.v(), m2, None, ALU.is_ge)
                    t.ts(nm1, m1, -1.0, None, ALU.mult)
                    t.act(L2.v(), LG.v(), AF.Exp, bias=nm1, scale=1.0)
                    t.tt(L2.v(), L2.v(), EQ.v(), ALU.mult)
                    t.op("dve", (lambda o, i: (lambda e: e.tensor_reduce(o, i, AX.X, ALU.add)))(ssum.ap, L2.t[:]),
                         reads=[L2.v()], writes=[ssum])
                    t.op("dve", (lambda o, i: (lambda e: e.reciprocal(o, i)))(rs.ap, ssum.ap), reads=[ssum], writes=[rs])
                    t.ts(GT.v(), L2.v(), rs, None, ALU.mult)
                    for e_ in range(8):
                        rp = REP.next()
                        t.ts(rp.v(), onesfull.v(), GT.v((slice(None), slice(e_, e_ + 1))), None, ALU.mult)
                        GP = ps.next()
                        t.mm(GP.v((slice(None), slice(0, 128))), rp.v(), ident.v())
                        t.cp(GATE.v((slice(None), e_, slice(j * 128, (j + 1) * 128)), e_),
                             GP.v((slice(None), slice(0, 128))), eng="act")
            for kc in range(KC):
                xv = X.v((slice(None), kc, slice(None)), kc)
                t.act(xv, xv, AF.Copy, scale=DN_ALPHA)
            for e_ in range(NE):
                w13 = [w1.t[e_].rearrange("(kc p) n -> p kc n", p=128), w3.t[e_].rearrange("(kc p) n -> p kc n", p=128)]
                w2e = w2.t[e_].rearrange("(c p) n -> p c n", p=128)
                for s in range(8):
                    Wb = []
                    for wi in range(2):
                        W = WS.next()
                        Wv = W.t[:, :].rearrange("p (kc n) -> p kc n", kc=KC)
                        t.dma("pool", V(Wv, W.res), V(w13[wi][:, :, s * 256:(s + 1) * 256], w1.res), W.v())
                        Wb.append((W, Wv))
                    for c in range(2):
                        P1 = ps.next()
                        P3 = ps.next()
                        for kc in range(KC):
                            t.mm(P1.v(), V(Wb[0][1][:, kc, c * 128:(c + 1) * 128], Wb[0][0].res),
                                 A.v((slice(None), kc, slice(None)), kc), start=(kc == 0), stop=(kc == KC - 1))
                        for kc in range(KC):
                            t.mm(P3.v(), V(Wb[1][1][:, kc, c * 128:(c + 1) * 128], Wb[1][0].res),
                                 A.v((slice(None), kc, slice(None)), kc), start=(kc == 0), stop=(kc == KC - 1))
                        sg = sgrot.next()
                        t.act(sg.v(), P1.v(), AF.Silu)
                        if moe:
                            t.tt(sg.v(), sg.v(), GATE.v((slice(None), e_, slice(None)), e_), ALU.mult)
                        hc = s * 2 + c
                        t.tt(H.v((slice(None), hc, slice(None)), hc), sg.v(), P3.v(), ALU.mult)
                for ds in range(8):
                    W = WS.next()
                    Wv = W.t[:, :].rearrange("p (c n) -> p c n", c=16)
                    t.dma("pool", V(Wv, W.res), V(w2e[:, :, ds * 512:(ds + 1) * 512], w2.res), W.v())
                    for j in range(4):
                        dt_ = ds * 4 + j
                        P = ps.next()
                        for c in range(16):
                            t.mm(P.v(), V(Wv[:, c, j * 128:(j + 1) * 128], W.res),
                                 H.v((slice(None), c, slice(None)), c), start=(c == 0), stop=(c == 15))
                        xv = X.v((slice(None), dt_, slice(None)), dt_)
                        t.tt(xv, xv, P.v(), ALU.add)
            emit_ln_fm(t, X, TG, ps, ones, lnp, 2, 3, sqrot, meanB, rstdB, tmpB, epsb.v())
            for half in range(2):
                ks = slice(half * 16, half * 16 + 16)
                t.dma("sp", V(yT3[:, ks, t0:t0 + TG], yT.res), V(X.t[:, ks, :], X.res[half * 16:half * 16 + 16]),
                      V(X.t[:, ks, :], [X.res[half * 16]]))
        t.emit()
    return nc


NFM = 11
NTM = 256


def build_A(T, stage=3):
    TT = 256
    nc = bass.Bass("TRN2", target_bir_lowering=False)
    es = ExitStack()
    with es:
        t = TR(nc, es)
        xT = t.dram("xT", [D_MODEL, T], F32, "ExternalInput")
        wfm_d = t.dram("wfm", [D_MODEL, NFM * 128], F32, "ExternalInput")
        wtm_d = t.dram("wtm", [D_MODEL, NTM], F32, "ExternalInput")
        convw_d = t.dram("convw", [128, 6, 4], F32, "ExternalInput")
        hp_d = t.dram("hp", [128, 8], F32, "ExternalInput")
        cst_d = t.dram("cst", [128, 6, 128], F32, "ExternalInput")
        alibi_d = t.dram("alibi", [128, 2, 256], F32, "ExternalInput")
        outT = t.dram("outT", [512, T], F32, "ExternalOutput")

        WFM = t.sbuf("WFM", [128, KC, NFM * 128], BF16)
        WTM = t.sbuf("WTM", [128, KC, NTM], BF16)
        XB = Rot([t.sbuf(f"XB{i}", [128, KC, TT], BF16) for i in range(2)])
        convw = t.sbuf("convw_s", [128, 6, 4], F32)
        hp = t.sbuf("hp_s", [128, 8], F32)
        cst = t.sbuf("cst_s", [128, 6, 128], F32)
        alibi = t.sbuf("alibi_s", [128, 2, 256], F32)
        ps = Rot([t.psum(f"ps{i}", [128, 512], F32) for i in range(8)])

        def c_(i):
            return cst.v((slice(None), i, slice(None)))
        IDENT, ONES, ONES128, TRI, NEGM, STRICT = [c_(i) for i in range(6)]

        PRE = [t.sbuf(f"PRE{i}", [128, 3 + TT], F32) for i in range(6)]
        CV = [t.sbuf(f"CV{i}", [128, TT], F32) for i in range(6)]
        ZT = [t.sbuf(f"ZT{i}", [128, TT], F32) for i in range(2)]
        SQT = [t.sbuf(f"SQT{i}", [128, TT], BF16) for i in range(2)]
        SKT = t.sbuf("SKT", [128, 128 + TT], BF16)
        SVr = Rot([t.sbuf(f"SV{i}", [128, 128], BF16) for i in range(3)])
        BAr = Rot([t.sbuf(f"BA{i}", [128, 132], F32) for i in range(2)])
        cacc = t.sbuf("cacc", [128, TT], F32)
        tmpA = Rot([t.sbuf(f"tmpA{i}", [128, TT], F32) for i in range(2)])
        smr = Rot([t.sbuf(f"smA{i}", [128, 24], F32, nres=24) for i in range(2)])
        S = [t.sbuf(f"S{i}", [128, 128], F32) for i in range(2)]
        class Roles:
            def __init__(self):
                self.d = {}

            def get(self, name, n=2):
                if name not in self.d:
                    self.d[name] = Rot([t.sbuf(f"r_{name}{i}", [128, 128], F32) for i in range(n)])
                return self.d[name].next()
        roles = Roles()
        OUTA = [Rot([t.sbuf(f"OUTA{h}_{i}", [128, TT], F32) for i in range(2)]) for h in range(2)]
        OUTB = [Rot([t.sbuf(f"OUTB{h}_{i}", [128, TT], F32) for i in range(2)]) for h in range(2)]
        Lr = Rot([t.sbuf(f"L{i}", [128, 256], F32) for i in range(2)])
        PTr = Rot([t.sbuf(f"PT{i}", [128, 128], BF16) for i in range(4)])
        NAL = t.sbuf("NAL", [128, 2], F32)
        eps6 = t.sbuf("eps6", [128, 1], F32)
        t.memset(eps6.v(), 1e-6)

        for (sb, dr) in ((convw, convw_d), (hp, hp_d), (cst, cst_d), (alibi, alibi_d)):
            t.dma("sp", sb.v(), dr.v(), sb.v())
        wfm3 = wfm_d.t.rearrange("(kc p) n -> p kc n", p=128)
        wtm3 = wtm_d.t.rearrange("(kc p) n -> p kc n", p=128)
        for q4 in range(4):
            ks = slice(q4 * 8, q4 * 8 + 8)
            t.dma("pool", V(WFM.t[:, ks, :], WFM.res), V(wfm3[:, ks, :], wfm_d.res), WFM.v())
        t.dma("pool", WTM.v(), V(wtm3, wtm_d.res), WTM.v())
        for i in range(6):
            t.memset(PRE[i].v((slice(None), slice(0, 3))), 0.0)
        for i in range(2):
            t.memset(S[i].v(), 0.0)
        t.act(NAL.v(), hp.v((slice(None), slice(0, 2))), AF.Exp)
        t.ts(NAL.v(), NAL.v(), -1.0, None, ALU.mult)

        xT3 = xT.t.rearrange("(kc p) t -> p kc t", p=128)
        SVprev = None
        for it in range(T // TT):
            t0 = it * TT
            xb = XB.next()
            for half in range(2):
                ks = slice(half * 16, half * 16 + 16)
                t.dma("pool", V(xb.t[:, ks, :], xb.res), V(xT3[:, ks, t0:t0 + TT], xT.res), xb.v())
            for ci in range(NFM):
                P = ps.next()
                pv = P.v((slice(None), slice(0, TT)))
                for kc in range(KC):
                    t.mm(pv, WFM.v((slice(None), kc, slice(ci * 128, (ci + 1) * 128))),
                         xb.v((slice(None), kc, slice(None))), start=(kc == 0), stop=(kc == KC - 1))
                if ci < 6:
                    t.cp(PRE[ci].v((slice(None), slice(3, 3 + TT))), pv, eng="act")
                elif ci < 8:
                    t.act(ZT[ci - 6].v(), pv, AF.Silu)
                elif ci < 10:
                    t.cp(SQT[ci - 8].v(), pv, eng="act")
                else:
                    t.cp(SKT.v((slice(None), slice(128, 128 + TT))), pv, eng="act")
            BA = []
            SV = []
            for bl in range(TT // 128 if stage >= 0.5 else 0):
                P = ps.next()
                pv = P.v((slice(None), slice(0, NTM)))
                for kc in range(KC):
                    t.mm(pv, xb.v((slice(None), kc, slice(bl * 128, (bl + 1) * 128))),
                         WTM.v((slice(None), kc, slice(None))), start=(kc == 0), stop=(kc == KC - 1))
                ba = BAr.next()
                sv = SVr.next()
                t.cp(ba.v(), P.v((slice(None), slice(0, 132))), eng="act")
                t.cp(sv.v(), ba.v((slice(None), slice(0, 128))), eng="act")
                BA.append(ba)
                SV.append(sv)
            for ci in range(6 if stage >= 1 else 0):
                pr = PRE[ci]
                t.ts(cacc.v(), pr.v((slice(None), slice(0, TT))), convw.v((slice(None), ci, slice(0, 1))), None, ALU.mult)
                for k in range(1, 4):
                    t.stt(cacc.v(), pr.v((slice(None), slice(k, k + TT))), convw.v((slice(None), ci, slice(k, k + 1))),
                          cacc.v(), ALU.mult, ALU.add)
                t.act(CV[ci].v(), cacc.v(), AF.Silu)
                t.cp(pr.v((slice(None), slice(0, 3))), pr.v((slice(None), slice(TT, TT + 3))))
                if ci < 4:
                    tq = tmpA.next()
                    t.act(tq.v(), CV[ci].v(), AF.Square)
                    P = ps.next()
                    pv = P.v((slice(None), slice(0, TT)))
                    t.mm(pv, ONES, tq.v())
                    t.act(tq.v(), pv, AF.Ln, bias=eps6.v(), scale=1.0)
                    t.act(tq.v(), tq.v(), AF.Exp, scale=-0.5)
                    t.tt(CV[ci].v(), CV[ci].v(), tq.v(), ALU.mult)
            outa = [OUTA[h].next() for h in range(2)]
            outb = [OUTB[h].next() for h in range(2)]
            if stage <= 1:
                for h in range(2):
                    if stage == 1:
                        t.cp(outa[h].v(), CV[h].v())
                        t.cp(outb[h].v(), CV[2 + h].v())
                    else:
                        t.cp(outa[h].v(), PRE[h].v((slice(None), slice(3, 3 + TT))))
                        t.cp(outb[h].v(), ZT[h].v())
            for bl in range(TT // 128 if stage >= 2 else 0):
                nb = (t0 // 128) + bl
                bs = slice(bl * 128, (bl + 1) * 128)
                sm = smr.next()

                def sc(i, n=1):
                    return sm.v((slice(None), slice(i, i + n)), list(range(i, i + n)))
                ba = BA[bl]
                BETA = sc(0, 2)
                NBETA = sc(2, 2)
                Gv = sc(4, 2)
                t.act(BETA, ba.v((slice(None), slice(128, 130))), AF.Sigmoid)
                t.ts(NBETA, BETA, -1.0, None, ALU.mult)
                xa = sc(6, 2)
                t.tt(xa, ba.v((slice(None), slice(130, 132))), hp.v((slice(None), slice(2, 4))), ALU.add)
                ab = sc(8, 2)
                t.stt(ab, xa, -1.0, xa, ALU.mult, ALU.max)
                t.act(ab, ab, AF.Exp, scale=-1.0)
                t.ts(ab, ab, 1.0, None, ALU.add)
                t.act(ab, ab, AF.Ln)
                t.ts(xa, xa, 0.0, None, ALU.max)
                t.tt(xa, xa, ab, ALU.add)
                t.tt(Gv, xa, NAL.v(), ALU.mult)
                P = ps.next()
                t.mm(P.v((slice(None), slice(0, 2))), TRI, Gv)
                GC = sc(10, 2)
                NGC = sc(12, 2)
                EGC = sc(14, 2)
                t.cp(GC, P.v((slice(None), slice(0, 2))), eng="act")
                t.ts(NGC, GC, -1.0, None, ALU.mult)
                t.act(EGC, GC, AF.Exp)
                for hd in range(2 if stage >= 3 else 0):
                    KT = CV[2 + hd].v((slice(None), bs))
                    QT = CV[0 + hd].v((slice(None), bs))
                    VT = CV[4 + hd].v((slice(None), bs))
                    beta = sc(0 + hd)
                    nbeta = sc(2 + hd)
                    g = sc(4 + hd)
                    gc = sc(10 + hd)
                    ngc = sc(12 + hd)
                    egc = sc(14 + hd)
                    GL = sc(16 + hd)
                    EGL = sc(18 + hd)
                    EKD = sc(20 + hd)
                    GREP = roles.get("GREP")
                    t.ts(GREP.v(), ONES, g, None, ALU.mult)
                    PG = ps.next()
                    pg = PG.v((slice(None), slice(0, 128)))
                    t.mm(pg, GREP.v(), TRI)
                    GBs = roles.get("GBs")
                    t.cp(GBs.v(), pg, eng="act")
                    pg = GBs.v()
                    t.cp(GL, GBs.v((slice(None), slice(127, 128))))
                    t.act(EGL, GL, AF.Exp)
                    t.act(EKD, gc, AF.Exp, bias=GL, scale=-1.0)
                    TMPD = roles.get("TMPD")
                    t.tt(TMPD.v(), pg, NEGM, ALU.add)
                    DECT = roles.get("DECT")
                    t.act(DECT.v(), TMPD.v(), AF.Exp, bias=ngc, scale=1.0)
                    EGB = roles.get("EGB")
                    t.act(EGB.v(), pg, AF.Exp)
                    PK = ps.next()
                    pk = PK.v((slice(None), slice(0, 128)))
                    t.mm(pk, KT, KT)
                    PQ = ps.next()
                    pq = PQ.v((slice(None), slice(0, 128)))
                    t.mm(pq, KT, QT)
                    NT = roles.get("NT")
                    t.tt(NT.v(), pk, DECT.v(), ALU.mult)
                    t.stt(NT.v(), NT.v(), beta, STRICT, ALU.mult, ALU.mult)
                    AQKT = roles.get("AQKT")
                    t.stt(AQKT.v(), pq, QSCALE, DECT.v(), ALU.mult, ALU.mult)
                    PM = ps.next()
                    pm = PM.v((slice(None), slice(0, 128)))
                    t.tp(pm, NT.v(), IDENT)
                    M = roles.get("M")
                    t.cp(M.v(), pm, eng="act")
                    PT_ = roles.get("PT_")
                    t.tt(PT_.v(), IDENT, NT.v(), ALU.subtract)
                    Nk, Mk = NT, M
                    for lvl in range(6):
                        PA = ps.next()
                        pa = PA.v((slice(None), slice(0, 128)))
                        t.mm(pa, Mk.v(), Nk.v())
                        PB = ps.next()
                        pb = PB.v((slice(None), slice(0, 128)))
                        t.mm(pb, Nk.v(), Mk.v())
                        N2 = roles.get("N2")
                        M2 = roles.get("M2")
                        t.cp(N2.v(), pa, eng="act")
                        t.cp(M2.v(), pb)
                        PC = ps.next()
                        pc = PC.v((slice(None), slice(0, 128)))
                        t.mm(pc, M2.v(), PT_.v())
                        PN_ = roles.get("PN_")
                        t.tt(PN_.v(), pc, PT_.v(), ALU.add)
                        PT_ = PN_
                        Nk, Mk = N2, M2
                    TTm = PT_
                    PKT = ps.next()
                    pkt = PKT.v((slice(None), slice(0, 128)))
                    t.tp(pkt, KT, IDENT)
                    XK = roles.get("XK")
                    KD = roles.get("KD")
                    t.act(XK.v(), pkt, AF.Copy, scale=egc)
                    t.act(KD.v(), pkt, AF.Copy, scale=EKD)
                    PVT = ps.next()
                    pvt = PVT.v((slice(None), slice(0, 128)))
                    t.tp(pvt, VT, IDENT)
                    XV = roles.get("XV")
                    t.cp(XV.v(), pvt, eng="act")
                    PW = ps.next()
                    pw = PW.v((slice(None), slice(0, 128)))
                    t.mm(pw, XK.v(), TTm.v())
                    WT = roles.get("WT")
                    t.cp(WT.v(), pw, eng="act")
                    PU = ps.next()
                    pu = PU.v((slice(None), slice(0, 128)))
                    t.mm(pu, TTm.v(), XV.v())
                    UP = roles.get("UP")
                    t.ts(UP.v(), pu, beta, None, ALU.mult)
                    QDT = roles.get("QDT")
                    t.stt(QDT.v(), QT, QSCALE, EGB.v(), ALU.mult, ALU.mult)
                    P1 = ps.next()
                    p1 = P1.v((slice(None), slice(0, 128)))
                    t.mm(p1, WT.v(), S[hd].v())
                    VN = roles.get("VN")
                    t.stt(VN.v(), p1, nbeta, UP.v(), ALU.mult, ALU.add)
                    P2 = ps.next()
                    p2 = P2.v((slice(None), slice(0, 128)))
                    t.mm(p2, S[hd].v(), QDT.v(), start=True, stop=False)
                    t.mm(p2, VN.v(), AQKT.v(), start=False, stop=True)
                    P3 = ps.next()
                    p3 = P3.v((slice(None), slice(0, 128)))
                    t.mm(p3, KD.v(), VN.v())
                    t.stt(S[hd].v(), S[hd].v(), EGL, p3, ALU.mult, ALU.add)
                    OT = roles.get("OT")
                    t.cp(OT.v(), p2, eng="act")
                    SQO = roles.get("SQO")
                    t.act(SQO.v(), p2, AF.Square)
                    PR = ps.next()
                    pr_ = PR.v((slice(None), slice(0, 128)))
                    t.mm(pr_, ONES128, SQO.v())
                    RS = roles.get("RS")
                    t.act(RS.v(), pr_, AF.Ln, bias=eps6.v(), scale=1.0)
                    t.act(RS.v(), RS.v(), AF.Exp, scale=-0.5)
                    t.stt(OT.v(), OT.v(), hp.v((slice(None), slice(6, 7))), RS.v(), ALU.mult, ALU.mult)
                    t.tt(outa[hd].v((slice(None), bs)), OT.v(), ZT[hd].v((slice(None), bs)), ALU.mult)
                svc = SV[bl]
                for h in range(2):
                    PS_ = ps.next()
                    L = Lr.next()
                    if nb > 0:
                        lo = 0
                    else:
                        lo = 128
                    n = 256 - lo
                    pv = PS_.v((slice(None), slice(0, n)))
                    t.mm(pv, SQT[h].v((slice(None), bs)),
                         SKT.v((slice(None), slice(bl * 128 + lo, bl * 128 + 256))))
                    lv = L.v((slice(None), slice(0, n)))
                    t.stt(lv, pv, QSCALE, alibi.v((slice(None), h, slice(lo, 256))), ALU.mult, ALU.add)
                    mx = sc(22)
                    nm = sc(23)
                    t.rmax(mx, lv)
                    t.tt(mx, mx, hp.v((slice(None), slice(4 + h, 5 + h))), ALU.max)
                    t.ts(nm, mx, -1.0, None, ALU.mult)
                    rs = sc(22)
                    es_ = sc(8)
                    t.act(lv, lv, AF.Exp, bias=nm, scale=1.0)
                    t.op("dve", (lambda o, i: (lambda e: e.tensor_reduce(o, i, AX.X, ALU.add)))(rs.ap, lv.ap),
                         reads=[lv], writes=[rs])
                    t.act(es_, hp.v((slice(None), slice(4 + h, 5 + h))), AF.Exp, bias=nm, scale=1.0)
                    t.tt(rs, rs, es_, ALU.add)
                    t.op("dve", (lambda o, i: (lambda e: e.reciprocal(o, i)))(rs.ap, rs.ap), reads=[rs], writes=[rs])
                    t.ts(lv, lv, rs, None, ALU.mult)
                    pts = []
                    for hf in range(n // 128):
                        PTP = ps.next()
                        ptp = PTP.v((slice(None), slice(0, 128)))
                        t.tp(ptp, L.v((slice(None), slice(hf * 128, (hf + 1) * 128))), IDENT)
                        pt = PTr.next()
                        t.cp(pt.v(), ptp, eng="act")
                        pts.append(pt)
                    PO = ps.next()
                    po = PO.v((slice(None), slice(0, 128)))
                    if nb > 0:
                        t.mm(po, SVprev.v(), pts[0].v(), start=True, stop=False)
                        t.mm(po, svc.v(), pts[1].v(), start=False, stop=True)
                    else:
                        t.mm(po, svc.v(), pts[0].v(), start=True, stop=True)
                    t.cp(outb[h].v((slice(None), bs)), po, eng="act")
                SVprev = svc
            t.cp(SKT.v((slice(None), slice(0, 128))), SKT.v((slice(None), slice(TT, TT + 128))))
            for h in range(2):
                t.dma("sp", V(outT.t[h * 128:(h + 1) * 128, t0:t0 + TT], outT.res), outa[h].v(), outa[h].v())
                t.dma("sp", V(outT.t[256 + h * 128:256 + (h + 1) * 128, t0:t0 + TT], outT.res), outb[h].v(), outb[h].v())
        t.emit()
    return nc


def _consts():
    i = np.arange(128)
    ident = np.eye(128, dtype=np.float32)
    ones = np.ones((128, 128), np.float32)
    ones128 = np.full((128, 128), 1.0 / 128, np.float32)
    tri = (i[:, None] <= i[None, :]).astype(np.float32)
    negm = np.where(i[:, None] <= i[None, :], 0.0, NEG).astype(np.float32)
    strict = (i[:, None] < i[None, :]).astype(np.float32)
    return np.ascontiguousarray(np.stack([ident, ones, ones128, tri, negm, strict], axis=1))


def _alibi(core):
    q = np.arange(128)[:, None]
    k = np.arange(256)[None, :]
    dist = q - k + 128
    valid = (dist >= 0) & (dist < 128)
    out = np.zeros((128, 2, 256), np.float32)
    for h in range(2):
        hq = 2 * core + h
        slope = np.float32(2.0) ** np.float32(-8.0 * (hq + 1) / 16)
        out[:, h, :] = np.where(valid, -slope * dist.astype(np.float32), np.float32(NEG))
    return out


def _pp(vec):
    return np.ascontiguousarray(vec.reshape(KC, 128).T)


_NC_CACHE = {}


def _get(kind, T):
    key = (kind, T)
    if key not in _NC_CACHE:
        if kind == "A":
            _NC_CACHE[key] = build_A(T)
        else:
            _NC_CACHE[key] = build_B(T, kind == "Bm")
    return _NC_CACHE[key]


def a_inputs(xT, w_in, conv_w, a_log, dt_bias, dn_norm_w, sinks, core):
    c = core
    cols = []
    for base in (0, 2048, 4096):
        for h in (2 * c, 2 * c + 1):
            cols.append(np.arange(base + h * 128, base + (h + 1) * 128))
    for h in (2 * c, 2 * c + 1):
        cols.append(np.arange(6144 + h * 128, 6144 + (h + 1) * 128))
    sq0 = 6144 + 2048 + 32
    for h in (2 * c, 2 * c + 1):
        cols.append(np.arange(sq0 + h * 128, sq0 + (h + 1) * 128))
    sk0 = sq0 + 2048
    kv = c // 2
    cols.append(np.arange(sk0 + kv * 128, sk0 + (kv + 1) * 128))
    wfm = np.ascontiguousarray(w_in[:, np.concatenate(cols)])
    b0 = 6144 + 2048
    a0 = b0 + 16
    sv0 = sk0 + 512
    tcols = np.concatenate([np.arange(sv0 + kv * 128, sv0 + (kv + 1) * 128),
                            [b0 + 2 * c, b0 + 2 * c + 1, a0 + 2 * c, a0 + 2 * c + 1]])
    wtm = np.zeros((w_in.shape[0], NTM), np.float32)
    wtm[:, :132] = w_in[:, tcols]
    convw = np.zeros((128, 6, 4), np.float32)
    ci = 0
    for base in (0, 2048, 4096):
        for h in (2 * c, 2 * c + 1):
            convw[:, ci, :] = conv_w[:, base + h * 128:base + (h + 1) * 128].T
            ci += 1
    hp = np.zeros((128, 8), np.float32)
    hp[:, 0:2] = a_log[2 * c:2 * c + 2][None, :]
    hp[:, 2:4] = dt_bias[2 * c:2 * c + 2][None, :]
    hp[:, 4:6] = sinks[2 * c:2 * c + 2][None, :]
    hp[:, 6] = dn_norm_w
    return {"xT": xT, "wfm": wfm, "wtm": wtm, "convw": convw, "hp": hp, "cst": _consts(), "alibi": _alibi(c)}


def run_A(xT, w_in, conv_w, a_log, dt_bias, dn_norm_w, sinks):
    T = xT.shape[1]
    nc = _get("A", T)
    in_maps = [a_inputs(xT, w_in, conv_w, a_log, dt_bias, dn_norm_w, sinks, c) for c in range(NCORE)]
    res = run_bass_kernel_spmd(nc, in_maps, core_ids=list(range(NCORE)))
    mixT = np.empty((D_MODEL, T), np.float32)
    for c in range(NCORE):
        o = res.results[c]["outT"]
        mixT[2 * c * 128:(2 * c + 2) * 128] = o[0:256]
        mixT[2048 + 2 * c * 128:2048 + (2 * c + 2) * 128] = o[256:512]
    return mixT


NB = 2


def run_B(xT, mixT, w_o, g1, b1, g2, b2, ffn, moe):
    T = xT.shape[1]
    Tc = T // NB
    nc = _get("Bm" if moe else "Bd", Tc)
    lnp = np.ascontiguousarray(np.stack([_pp(g1), _pp(b1), _pp(g2), _pp(b2)], axis=1))
    ones = np.full((128, 128), 1.0 / D_MODEL, np.float32)
    ident = np.eye(128, dtype=np.float32)
    base = {"w_o": w_o, "lnp": lnp, "onesd": ones, "ident": ident}
    if moe:
        rw, ew1, ew3, ew2 = ffn
        base.update({"rw": np.ascontiguousarray(rw.reshape(KC, 128, 8).transpose(1, 0, 2)), "w1": ew1, "w3": ew3, "w2": ew2})
    else:
        fw1, fw3, fw2 = ffn
        base.update({"w1": np.ascontiguousarray(fw1.reshape(D_MODEL, 4, 2048).transpose(1, 0, 2)),
                     "w3": np.ascontiguousarray(fw3.reshape(D_MODEL, 4, 2048).transpose(1, 0, 2)),
                     "w2": fw2.reshape(4, 2048, D_MODEL)})
    in_maps = []
    for c in range(NB):
        m = dict(base)
        m["xT"] = np.ascontiguousarray(xT[:, c * Tc:(c + 1) * Tc])
        m["mixT"] = np.ascontiguousarray(mixT[:, c * Tc:(c + 1) * Tc])
        in_maps.append(m)
    res = run_bass_kernel_spmd(nc, in_maps, core_ids=list(range(NB)))
    return np.concatenate([res.results[c]["yT"] for c in range(NB)], axis=1)


def kernel(x, w_in, conv_w, a_log, dt_bias, dn_norm_w, sinks, w_o, ln1_g, ln1_b,
           ffn_w1, ffn_w3, ffn_w2, router_w, exp_w1, exp_w3, exp_w2, ln2_g, ln2_b):
    f = lambda a: np.asarray(a, dtype=np.float32)
    xT = np.ascontiguousarray(f(x)[0].T)
    for i in range(DEPTH):
        mixT = run_A(xT, f(w_in[i]), f(conv_w[i]), f(a_log[i]), f(dt_bias[i]), f(dn_norm_w[i]), f(sinks[i]))
        j = i // 2
        if i % 2 == 0:
            ffn = (f(ffn_w1[j]), f(ffn_w3[j]), f(ffn_w2[j]))
        else:
            ffn = (f(router_w[j]), f(exp_w1[j]), f(exp_w3[j]), f(exp_w2[j]))
        xT = run_B(xT, mixT, f(w_o[i]), f(ln1_g[i]), f(ln1_b[i]), f(ln2_g[i]), f(ln2_b[i]), ffn, i % 2 == 1)
    return np.ascontiguousarray(xT.T)[None].astype(np.float32)
```

```python
import numpy as np
from contextlib import ExitStack
import concourse.bass as bass
import concourse.mybir as mybir
from concourse.bass_utils import run_bass_kernel_spmd

F32 = mybir.dt.float32
BF16 = mybir.dt.bfloat16
AF = mybir.ActivationFunctionType
ALU = mybir.AluOpType
AX = mybir.AxisListType

D_MODEL = 4096
SEQ = 16384
DEPTH = 4
NCORE = 8
KC = D_MODEL // 128
DN_ALPHA = (2 * DEPTH) ** 0.25
LN_EPS = 1e-5
NEG = -1e30
QSCALE = 128 ** -0.5


class Res:
    __slots__ = ("name", "lw", "rd", "sem", "cnt")

    def __init__(self, name):
        self.name = name
        self.lw = None
        self.rd = []
        self.sem = None
        self.cnt = 0


class V:
    __slots__ = ("ap", "res")

    def __init__(self, ap, res):
        self.ap = ap
        self.res = res


class Buf:
    def __init__(self, name, t, nres=1):
        self.name = name
        self.t = t
        self.res = [Res(f"{name}.{i}") for i in range(nres)]

    def v(self, idx=None, r=None):
        ap = self.t[:] if idx is None else self.t[idx]
        if r is None:
            rs = self.res
        elif isinstance(r, int):
            rs = [self.res[r]]
        else:
            rs = [self.res[i] for i in r]
        return V(ap, rs)


class Op:
    __slots__ = ("id", "eng", "fn", "deps", "dma", "semres", "tok", "inc", "ms")


EPOCH = 20000


class TR:
    ENG = ("pe", "act", "dve", "pool", "sp")

    def __init__(self, nc, es):
        self.nc = nc
        self.es = es
        self.ops = []
        self.nbuf = 0

    def sbuf(self, name, shape, dt, nres=1):
        t = self.es.enter_context(self.nc.sbuf_tensor(name, list(shape), dt))
        return Buf(name, t, nres)

    def psum(self, name, shape, dt):
        t = self.es.enter_context(self.nc.psum_tensor(name, list(shape), dt))
        return Buf(name, t, 1)

    def dram(self, name, shape, dt, kind):
        t = self.nc.dram_tensor(name, list(shape), dt, kind=kind).ap()
        return Buf(name, t, 1)

    def op(self, eng, fn, reads=(), writes=(), dma=False, semres=None):
        o = Op()
        o.id = len(self.ops)
        o.eng = eng
        o.fn = fn
        o.dma = dma
        o.semres = semres
        o.inc = False
        o.ms = None
        o.tok = None
        deps = {}
        rres = []
        for v in reads:
            rres.extend(v.res)
        wres = []
        for v in writes:
            wres.extend(v.res)
        for r in rres:
            if r.lw is not None:
                deps[r.lw] = "raw"
        for w in wres:
            if w.lw is not None and w.lw not in deps:
                deps[w.lw] = "waw"
            for x in w.rd:
                if x not in deps:
                    deps[x] = "war"
        for r in rres:
            r.rd.append(o.id)
        for w in wres:
            w.lw = o.id
            w.rd = []
        keep = []
        for d, kind in deps.items():
            p = self.ops[d]
            if (not p.dma) and (not dma) and p.eng == eng:
                if eng == "pe":
                    continue
                if kind != "raw":
                    continue
            keep.append(d)
        o.deps = sorted(keep)
        self.ops.append(o)
        return o

    def dma(self, q, out, in_, semside):
        o_ap, i_ap = out.ap, in_.ap
        return self.op(q, lambda e, s, n: e.dma_start(out=o_ap, in_=i_ap).then_inc(s, 16),
                       reads=[in_], writes=[out], dma=True, semres=semside.res[0])

    def emit(self):
        nc = self.nc
        ops = self.ops
        for o in ops:
            if o.dma:
                r = o.semres
                if r.sem is None:
                    r.sem = self.es.enter_context(nc.semaphore(f"d{o.id}"))
                r.cnt += 16
                o.tok = (r.sem, r.cnt)
        waited = {e: {} for e in self.ENG}
        for o in ops:
            w = waited[o.eng]
            for d in o.deps:
                p = ops[d]
                if p.dma:
                    continue
                if w.get(p.eng, -1) < p.id:
                    p.inc = True
                    w[p.eng] = p.id
        cnt = {e: 0 for e in self.ENG}
        esems = {}
        for o in ops:
            if (not o.dma) and o.inc:
                k = cnt[o.eng]
                cnt[o.eng] += 1
                ep = k // EPOCH
                key = (o.eng, ep)
                if key not in esems:
                    esems[key] = self.es.enter_context(nc.semaphore(f"e_{o.eng}_{ep}"))
                o.tok = (esems[key], k % EPOCH + 1)
        streams = {e: [] for e in self.ENG}
        waited = {e: {} for e in self.ENG}
        for o in ops:
            w = waited[o.eng]
            st = streams[o.eng]
            for d in o.deps:
                p = ops[d]
                if p.tok is None:
                    continue
                sem, val = p.tok
                key = id(sem)
                if w.get(key, 0) < val:
                    w[key] = val
                    st.append(("w", sem, val))
            st.append(("o", o))
        seen = {}
        for o in ops:
            if o.dma:
                seen[id(o.semres.sem)] = (o.semres.sem, o.semres.cnt)
        for sem, val in seen.values():
            streams["sp"].append(("w", sem, val))

        def run(eng, st):
            for it in st:
                if it[0] == "w":
                    eng.wait_ge(it[1], it[2])
                else:
                    o = it[1]
                    if o.dma:
                        o.fn(eng, o.tok[0], 16)
                    else:
                        ins = o.fn(eng)
                        if o.inc:
                            ins.then_inc(o.tok[0], 1)

        with nc.Block() as block:
            @block.tensor
            def _(e):
                run(e, streams["pe"])

            @block.scalar
            def _(e):
                run(e, streams["act"])

            @block.vector
            def _(e):
                run(e, streams["dve"])

            @block.gpsimd
            def _(e):
                run(e, streams["pool"])

            @block.sync
            def _(e):
                run(e, streams["sp"])

    def mm(self, out, lhsT, rhs, start=True, stop=True):
        o, l, r = out.ap, lhsT.ap, rhs.ap
        return self.op("pe", lambda e: e.matmul(o, l, r, start=start, stop=stop),
                       reads=[lhsT, rhs], writes=[out])

    def tp(self, out, in_, ident):
        o, i, d = out.ap, in_.ap, ident.ap
        return self.op("pe", lambda e: e.transpose(o, i, d), reads=[in_, ident], writes=[out])

    def act(self, out, in_, func, bias=None, scale=None, accum=None, eng="act"):
        o, i = out.ap, in_.ap
        kw = {}
        reads = [in_]
        writes = [out]
        if bias is not None:
            if isinstance(bias, V):
                kw["bias"] = bias.ap
                reads.append(bias)
            else:
                kw["bias"] = bias
        if scale is not None:
            if isinstance(scale, V):
                kw["scale"] = scale.ap
                reads.append(scale)
            else:
                kw["scale"] = scale
        if accum is not None:
            kw["accum_out"] = accum.ap
            writes.append(accum)
        return self.op(eng, lambda e: e.activation(o, i, func, **kw), reads=reads, writes=writes)

    def ts(self, out, in0, s1, s2, op0, op1=None, eng="dve"):
        o, i = out.ap, in0.ap
        reads = [in0]
        a1 = s1
        a2 = s2
        if isinstance(s1, V):
            a1 = s1.ap
            reads.append(s1)
        if isinstance(s2, V):
            a2 = s2.ap
            reads.append(s2)
        if op1 is None:
            return self.op(eng, lambda e: e.tensor_scalar(o, i, a1, None, op0), reads=reads, writes=[out])
        return self.op(eng, lambda e: e.tensor_scalar(o, i, a1, a2, op0, op1), reads=reads, writes=[out])

    def stt(self, out, in0, s, in1, op0, op1, eng="dve"):
        o, i0, i1 = out.ap, in0.ap, in1.ap
        reads = [in0, in1]
        a = s
        if isinstance(s, V):
            a = s.ap
            reads.append(s)
        return self.op(eng, lambda e: e.scalar_tensor_tensor(o, i0, a, i1, op0, op1), reads=reads, writes=[out])

    def tt(self, out, in0, in1, op, eng="dve"):
        o, i0, i1 = out.ap, in0.ap, in1.ap
        return self.op(eng, lambda e: e.tensor_tensor(o, i0, i1, op), reads=[in0, in1], writes=[out])

    def cp(self, out, in_, eng="dve"):
        o, i = out.ap, in_.ap
        if eng == "act":
            return self.op(eng, lambda e: e.copy(o, i), reads=[in_], writes=[out])
        return self.op(eng, lambda e: e.tensor_copy(o, i), reads=[in_], writes=[out])

    def memset(self, out, val, eng="dve"):
        o = out.ap
        return self.op(eng, lambda e: e.memset(o, val), reads=[], writes=[out])

    def rmax(self, out, in_):
        o, i = out.ap, in_.ap
        return self.op("dve", lambda e: e.tensor_reduce(o, i, AX.X, ALU.max), reads=[in_], writes=[out])


class Rot:
    def __init__(self, bufs):
        self.bufs = bufs
        self.i = 0

    def next(self):
        b = self.bufs[self.i % len(self.bufs)]
        self.i += 1
        return b


def emit_ln_fm(t, X, N, ps, ones, lnp, gi, bi, sqrot, meanB, rstdB, tmpB, epsv, post=None):
    S = ps.next()
    Q = ps.next()
    for kc in range(KC):
        t.mm(S.v((slice(None), slice(0, N))), ones.v(), X.v((slice(None), kc, slice(None)), kc),
             start=(kc == 0), stop=(kc == KC - 1))
    for kc in range(KC):
        sq = sqrot.next()
        t.act(sq.v(), X.v((slice(None), kc, slice(None)), kc), AF.Square)
        t.mm(Q.v((slice(None), slice(0, N))), ones.v(), sq.v(), start=(kc == 0), stop=(kc == KC - 1))
    t.cp(meanB.v(), S.v((slice(None), slice(0, N))), eng="act")
    t.tt(tmpB.v(), meanB.v(), meanB.v(), ALU.mult)
    t.tt(tmpB.v(), Q.v((slice(None), slice(0, N))), tmpB.v(), ALU.subtract)
    t.act(rstdB.v(), tmpB.v(), AF.Ln, bias=epsv, scale=1.0)
    t.act(rstdB.v(), rstdB.v(), AF.Exp, scale=-0.5)
    for kc in range(KC):
        xv = X.v((slice(None), kc, slice(None)), kc)
        t.tt(xv, xv, meanB.v(), ALU.subtract)
        t.tt(xv, xv, rstdB.v(), ALU.mult)
        t.ts(xv, xv, lnp.v((slice(None), gi, slice(kc, kc + 1))), lnp.v((slice(None), bi, slice(kc, kc + 1))),
             ALU.mult, ALU.add)
        if post is not None:
            post(kc, xv)


def build_B(T, moe):
    TG = 512
    NE = 8 if moe else 4
    nc = bass.Bass("TRN2", target_bir_lowering=False)
    es = ExitStack()
    with es:
        t = TR(nc, es)
        xT = t.dram("xT", [D_MODEL, T], F32, "ExternalInput")
        mixT = t.dram("mixT", [D_MODEL, T], F32, "ExternalInput")
        w_o = t.dram("w_o", [D_MODEL, D_MODEL], F32, "ExternalInput")
        lnp_d = t.dram("lnp", [128, 4, KC], F32, "ExternalInput")
        ones_d = t.dram("onesd", [128, 128], F32, "ExternalInput")
        ident_d = t.dram("ident", [128, 128], F32, "ExternalInput")
        w1 = t.dram("w1", [NE, D_MODEL, 2048], F32, "ExternalInput")
        w3 = t.dram("w3", [NE, D_MODEL, 2048], F32, "ExternalInput")
        w2 = t.dram("w2", [NE, 2048, D_MODEL], F32, "ExternalInput")
        if moe:
            rw_d = t.dram("rw", [128, KC, 8], F32, "ExternalInput")
        yT = t.dram("yT", [D_MODEL, T], F32, "ExternalOutput")

        X = t.sbuf("X", [128, KC, TG], F32, nres=KC)
        A = t.sbuf("A", [128, KC, TG], BF16, nres=KC)
        WS = Rot([t.sbuf(f"WS{i}", [128, 8192], BF16) for i in range(3 if moe else 4)])
        H = t.sbuf("H", [128, 16, TG], BF16, nres=16)
        lnp = t.sbuf("lnp_s", [128, 4, KC], F32)
        ones = t.sbuf("ones_s", [128, 128], F32)
        ident = t.sbuf("ident_s", [128, 128], F32)
        sqrot = Rot([t.sbuf(f"sq{i}", [128, TG], F32) for i in range(2)])
        meanB = t.sbuf("meanB", [128, TG], F32)
        rstdB = t.sbuf("rstdB", [128, TG], F32)
        tmpB = t.sbuf("tmpB", [128, TG], F32)
        sgrot = Rot([t.sbuf(f"sg{i}", [128, TG], F32) for i in range(2)])
        ps = Rot([t.psum(f"ps{i}", [128, 512], F32) for i in range(8)])
        if moe:
            RW = t.sbuf("RW", [128, KC, 8], F32)
            GATE = t.sbuf("GATE", [128, 8, TG], F32, nres=8)
            LG = t.sbuf("LG", [128, 8], F32)
            L2 = t.sbuf("L2", [128, 8], F32)
            EQ = t.sbuf("EQ", [128, 8], F32)
            GT = t.sbuf("GT", [128, 8], F32)
            sm = t.sbuf("sm", [128, 8], F32, nres=8)
            REP = Rot([t.sbuf(f"rep{i}", [128, 128], F32) for i in range(2)])
            onesfull = t.sbuf("onesfull", [128, 128], F32)
            t.dma("sp", RW.v(), rw_d.v(), RW.v())
            t.memset(onesfull.v(), 1.0)

        epsb = t.sbuf("epsb", [128, 1], F32)
        t.memset(epsb.v(), LN_EPS)
        t.dma("sp", lnp.v(), lnp_d.v(), lnp.v())
        t.dma("sp", ones.v(), ones_d.v(), ones.v())
        t.dma("sp", ident.v(), ident_d.v(), ident.v())

        xT3 = xT.t.rearrange("(kc p) t -> p kc t", p=128)
        mixT3 = mixT.t.rearrange("(kc p) t -> p kc t", p=128)
        yT3 = yT.t.rearrange("(kc p) t -> p kc t", p=128)
        wo3 = w_o.t.rearrange("(kc p) n -> p kc n", p=128)

        for g in range(T // TG):
            t0 = g * TG
            for half in range(2):
                ks = slice(half * 16, half * 16 + 16)
                t.dma("sp", V(X.t[:, ks, :], X.res[half * 16:half * 16 + 16]),
                      V(xT3[:, ks, t0:t0 + TG], xT.res), V(X.t[:, ks, :], [X.res[half * 16]]))
                t.dma("pool", V(A.t[:, ks, :], A.res[half * 16:half * 16 + 16]),
                      V(mixT3[:, ks, t0:t0 + TG], mixT.res), V(A.t[:, ks, :], [A.res[half * 16]]))
            for ds in range(D_MODEL // 256):
                W = WS.next()
                Wv = W.t[:, :].rearrange("p (kc n) -> p kc n", kc=KC)
                t.dma("pool", V(Wv, W.res), V(wo3[:, :, ds * 256:(ds + 1) * 256], w_o.res), W.v())
                for j in range(2):
                    dt_ = ds * 2 + j
                    P = ps.next()
                    for kc in range(KC):
                        t.mm(P.v(), V(Wv[:, kc, j * 128:(j + 1) * 128], W.res),
                             A.v((slice(None), kc, slice(None)), kc), start=(kc == 0), stop=(kc == KC - 1))
                    xv = X.v((slice(None), dt_, slice(None)), dt_)
                    t.stt(xv, xv, DN_ALPHA, P.v(), ALU.mult, ALU.add)
            def post1(kc, xv):
                t.cp(A.v((slice(None), kc, slice(None)), kc), xv, eng="act")
            emit_ln_fm(t, X, TG, ps, ones, lnp, 0, 1, sqrot, meanB, rstdB, tmpB, epsb.v(), post=post1)
            if moe:
                for j in range(TG // 128):
                    R = ps.next()
                    for kc in range(KC):
                        t.mm(R.v((slice(None), slice(0, 8))), X.v((slice(None), kc, slice(j * 128, (j + 1) * 128)), kc),
                             RW.v((slice(None), kc, slice(None))), start=(kc == 0), stop=(kc == KC - 1))
                    t.cp(LG.v(), R.v((slice(None), slice(0, 8))))
                    m1 = sm.v((slice(None), slice(0, 1)), 0)
                    m2 = sm.v((slice(None), slice(1, 2)), 1)
                    nm1 = sm.v((slice(None), slice(2, 3)), 2)
                    ssum = sm.v((slice(None), slice(3, 4)), 3)
                    rs = sm.v((slice(None), slice(4, 5)), 4)
                    t.rmax(m1, LG.v())
                    t.ts(EQ.v(), LG.v(), m1, NEG, ALU.is_equal, ALU.mult)
                    t.tt(L2.v(), LG.v(), EQ.v(), ALU.add)
                    t.rmax(m2, L2.v())
                    t.ts(EQ.v(), LG.v(), m2, None, ALU.is_ge)
                    t.ts(nm1, m1, -1.0, None, ALU.mult)
                    t.act(L2.v(), LG.v(), AF.Exp, bias=nm1, scale=1.0)
                    t.tt(L2.v(), L2.v(), EQ.v(), ALU.mult)
                    t.op("dve", (lambda o, i: (lambda e: e.tensor_reduce(o, i, AX.X, ALU.add)))(ssum.ap, L2.t[:]),
                         reads=[L2.v()], writes=[ssum])
                    t.op("dve", (lambda o, i: (lambda e: e.reciprocal(o, i)))(rs.ap, ssum.ap), reads=[ssum], writes=[rs])
                    t.ts(GT.v(), L2.v(), rs, None, ALU.mult)
                    for e_ in range(8):
                        rp = REP.next()
                        t.ts(rp.v(), onesfull.v(), GT.v((slice(None), slice(e_, e_ + 1))), None, ALU.mult)
                        GP = ps.next()
                        t.mm(GP.v((slice(None), slice(0, 128))), rp.v(), ident.v())
                        t.cp(GATE.v((slice(None), e_, slice(j * 128, (j + 1) * 128)), e_),
                             GP.v((slice(None), slice(0, 128))), eng="act")
            for kc in range(KC):
                xv = X.v((slice(None), kc, slice(None)), kc)
                t.act(xv, xv, AF.Copy, scale=DN_ALPHA)
            for e_ in range(NE):
                w13 = [w1.t[e_].rearrange("(kc p) n -> p kc n", p=128), w3.t[e_].rearrange("(kc p) n -> p kc n", p=128)]
                w2e = w2.t[e_].rearrange("(c p) n -> p c n", p=128)
                for s in range(8):
                    Wb = []
                    for wi in range(2):
                        W = WS.next()
                        Wv = W.t[:, :].rearrange("p (kc n) -> p kc n", kc=KC)
                        t.dma("pool", V(Wv, W.res), V(w13[wi][:, :, s * 256:(s + 1) * 256], w1.res), W.v())
                        Wb.append((W, Wv))
                    for c in range(2):
                        P1 = ps.next()
                        P3 = ps.next()
                        for kc in range(KC):
                            t.mm(P1.v(), V(Wb[0][1][:, kc, c * 128:(c + 1) * 128], Wb[0][0].res),
                                 A.v((slice(None), kc, slice(None)), kc), start=(kc == 0), stop=(kc == KC - 1))
                        for kc in range(KC):
                            t.mm(P3.v(), V(Wb[1][1][:, kc, c * 128:(c + 1) * 128], Wb[1][0].res),
                                 A.v((slice(None), kc, slice(None)), kc), start=(kc == 0), stop=(kc == KC - 1))
                        sg = sgrot.next()
                        t.act(sg.v(), P1.v(), AF.Silu)
                        if moe:
                            t.tt(sg.v(), sg.v(), GATE.v((slice(None), e_, slice(None)), e_), ALU.mult)
                        hc = s * 2 + c
                        t.tt(H.v((slice(None), hc, slice(None)), hc), sg.v(), P3.v(), ALU.mult)
                for ds in range(8):
                    W = WS.next()
                    Wv = W.t[:, :].rearrange("p (c n) -> p c n", c=16)
                    t.dma("pool", V(Wv, W.res), V(w2e[:, :, ds * 512:(ds + 1) * 512], w2.res), W.v())
                    for j in range(4):
                        dt_ = ds * 4 + j
                        P = ps.next()
                        for c in range(16):
                            t.mm(P.v(), V(Wv[:, c, j * 128:(j + 1) * 128], W.res),
                                 H.v((slice(None), c, slice(None)), c), start=(c == 0), stop=(c == 15))
                        xv = X.v((slice(None), dt_, slice(None)), dt_)
                        t.tt(xv, xv, P.v(), ALU.add)
            emit_ln_fm(t, X, TG, ps, ones, lnp, 2, 3, sqrot, meanB, rstdB, tmpB, epsb.v())
            for half in range(2):
                ks = slice(half * 16, half * 16 + 16)
                t.dma("sp", V(yT3[:, ks, t0:t0 + TG], yT.res), V(X.t[:, ks, :], X.res[half * 16:half * 16 + 16]),
                      V(X.t[:, ks, :], [X.res[half * 16]]))
        t.emit()
    return nc


NFM = 11
NTM = 256


def build_A(T, stage=3):
    TT = 256
    nc = bass.Bass("TRN2", target_bir_lowering=False)
    es = ExitStack()
    with es:
        t = TR(nc, es)
        xT = t.dram("xT", [D_MODEL, T], F32, "ExternalInput")
        wfm_d = t.dram("wfm", [D_MODEL, NFM * 128], F32, "ExternalInput")
        wtm_d = t.dram("wtm", [D_MODEL, NTM], F32, "ExternalInput")
        convw_d = t.dram("convw", [128, 6, 4], F32, "ExternalInput")
        hp_d = t.dram("hp", [128, 8], F32, "ExternalInput")
        cst_d = t.dram("cst", [128, 6, 128], F32, "ExternalInput")
        alibi_d = t.dram("alibi", [128, 2, 256], F32, "ExternalInput")
        outT = t.dram("outT", [512, T], F32, "ExternalOutput")

        WFM = t.sbuf("WFM", [128, KC, NFM * 128], BF16)
        WTM = t.sbuf("WTM", [128, KC, NTM], BF16)
        XB = Rot([t.sbuf(f"XB{i}", [128, KC, TT], BF16) for i in range(2)])
        convw = t.sbuf("convw_s", [128, 6, 4], F32)
        hp = t.sbuf("hp_s", [128, 8], F32)
        cst = t.sbuf("cst_s", [128, 6, 128], F32)
        alibi = t.sbuf("alibi_s", [128, 2, 256], F32)
        ps = Rot([t.psum(f"ps{i}", [128, 512], F32) for i in range(8)])

        def c_(i):
            return cst.v((slice(None), i, slice(None)))
        IDENT, ONES, ONES128, TRI, NEGM, STRICT = [c_(i) for i in range(6)]

        PRE = [t.sbuf(f"PRE{i}", [128, 3 + TT], F32) for i in range(6)]
        CV = [t.sbuf(f"CV{i}", [128, TT], F32) for i in range(6)]
        ZT = [t.sbuf(f"ZT{i}", [128, TT], F32) for i in range(2)]
        SQT = [t.sbuf(f"SQT{i}", [128, TT], BF16) for i in range(2)]
        SKT = t.sbuf("SKT", [128, 128 + TT], BF16)
        SVr = Rot([t.sbuf(f"SV{i}", [128, 128], BF16) for i in range(3)])
        BAr = Rot([t.sbuf(f"BA{i}", [128, 132], F32) for i in range(2)])
        cacc = t.sbuf("cacc", [128, TT], F32)
        tmpA = Rot([t.sbuf(f"tmpA{i}", [128, TT], F32) for i in range(2)])
        smr = Rot([t.sbuf(f"smA{i}", [128, 28], F32, nres=28) for i in range(2)])
        S = [t.sbuf(f"S{i}", [128, 128], F32) for i in range(2)]
        class Roles:
            def __init__(self):
                self.d = {}

            def get(self, name, n=2):
                if name not in self.d:
                    self.d[name] = Rot([t.sbuf(f"r_{name}{i}", [128, 128], F32) for i in range(n)])
                return self.d[name].next()
        roles = Roles()
        OUTA = [Rot([t.sbuf(f"OUTA{h}_{i}", [128, TT], F32) for i in range(2)]) for h in range(2)]
        OUTB = [Rot([t.sbuf(f"OUTB{h}_{i}", [128, TT], F32) for i in range(2)]) for h in range(2)]
        Lr = Rot([t.sbuf(f"L{i}", [128, 256], F32) for i in range(2)])
        PTr = Rot([t.sbuf(f"PT{i}", [128, 128], BF16) for i in range(4)])
        NAL = t.sbuf("NAL", [128, 2], F32)
        eps6 = t.sbuf("eps6", [128, 1], F32)
        t.memset(eps6.v(), 1e-6)

        for (sb, dr) in ((convw, convw_d), (hp, hp_d), (cst, cst_d), (alibi, alibi_d)):
            t.dma("sp", sb.v(), dr.v(), sb.v())
        wfm3 = wfm_d.t.rearrange("(kc p) n -> p kc n", p=128)
        wtm3 = wtm_d.t.rearrange("(kc p) n -> p kc n", p=128)
        for q4 in range(4):
            ks = slice(q4 * 8, q4 * 8 + 8)
            t.dma("pool", V(WFM.t[:, ks, :], WFM.res), V(wfm3[:, ks, :], wfm_d.res), WFM.v())
        t.dma("pool", WTM.v(), V(wtm3, wtm_d.res), WTM.v())
        for i in range(6):
            t.memset(PRE[i].v((slice(None), slice(0, 3))), 0.0)
        for i in range(2):
            t.memset(S[i].v(), 0.0)
        t.act(NAL.v(), hp.v((slice(None), slice(0, 2))), AF.Exp)
        t.ts(NAL.v(), NAL.v(), -1.0, None, ALU.mult)

        xT3 = xT.t.rearrange("(kc p) t -> p kc t", p=128)
        SVprev = None
        for it in range(T // TT):
            t0 = it * TT
            xb = XB.next()
            for half in range(2):
                ks = slice(half * 16, half * 16 + 16)
                t.dma("pool", V(xb.t[:, ks, :], xb.res), V(xT3[:, ks, t0:t0 + TT], xT.res), xb.v())
            for ci in range(NFM):
                P = ps.next()
                pv = P.v((slice(None), slice(0, TT)))
                for kc in range(KC):
                    t.mm(pv, WFM.v((slice(None), kc, slice(ci * 128, (ci + 1) * 128))),
                         xb.v((slice(None), kc, slice(None))), start=(kc == 0), stop=(kc == KC - 1))
                if ci < 6:
                    t.cp(PRE[ci].v((slice(None), slice(3, 3 + TT))), pv, eng="act")
                elif ci < 8:
                    t.act(ZT[ci - 6].v(), pv, AF.Silu)
                elif ci < 10:
                    t.cp(SQT[ci - 8].v(), pv, eng="act")
                else:
                    t.cp(SKT.v((slice(None), slice(128, 128 + TT))), pv, eng="act")
            BA = []
            SV = []
            for bl in range(TT // 128 if stage >= 0.5 else 0):
                P = ps.next()
                pv = P.v((slice(None), slice(0, NTM)))
                for kc in range(KC):
                    t.mm(pv, xb.v((slice(None), kc, slice(bl * 128, (bl + 1) * 128))),
                         WTM.v((slice(None), kc, slice(None))), start=(kc == 0), stop=(kc == KC - 1))
                ba = BAr.next()
                sv = SVr.next()
                t.cp(ba.v(), P.v((slice(None), slice(0, 132))), eng="act")
                t.cp(sv.v(), ba.v((slice(None), slice(0, 128))), eng="act")
                BA.append(ba)
                SV.append(sv)
            for ci in range(6 if stage >= 1 else 0):
                pr = PRE[ci]
                t.ts(cacc.v(), pr.v((slice(None), slice(0, TT))), convw.v((slice(None), ci, slice(0, 1))), None, ALU.mult)
                for k in range(1, 4):
                    t.stt(cacc.v(), pr.v((slice(None), slice(k, k + TT))), convw.v((slice(None), ci, slice(k, k + 1))),
                          cacc.v(), ALU.mult, ALU.add)
                t.act(CV[ci].v(), cacc.v(), AF.Silu)
                t.cp(pr.v((slice(None), slice(0, 3))), pr.v((slice(None), slice(TT, TT + 3))))
                if ci < 4:
                    tq = tmpA.next()
                    t.act(tq.v(), CV[ci].v(), AF.Square)
                    P = ps.next()
                    pv = P.v((slice(None), slice(0, TT)))
                    t.mm(pv, ONES, tq.v())
                    t.act(tq.v(), pv, AF.Ln, bias=eps6.v(), scale=1.0)
                    t.act(tq.v(), tq.v(), AF.Exp, scale=-0.5)
                    t.tt(CV[ci].v(), CV[ci].v(), tq.v(), ALU.mult)
            outa = [OUTA[h].next() for h in range(2)]
            outb = [OUTB[h].next() for h in range(2)]
            if stage <= 1:
                for h in range(2):
                    if stage == 1:
                        t.cp(outa[h].v(), CV[h].v())
                        t.cp(outb[h].v(), CV[2 + h].v())
                    else:
                        t.cp(outa[h].v(), PRE[h].v((slice(None), slice(3, 3 + TT))))
                        t.cp(outb[h].v(), ZT[h].v())
            for bl in range(TT // 128 if stage >= 2 else 0):
                nb = (t0 // 128) + bl
                bs = slice(bl * 128, (bl + 1) * 128)
                sm = smr.next()

                def sc(i, n=1):
                    return sm.v((slice(None), slice(i, i + n)), list(range(i, i + n)))
                ba = BA[bl]
                BETA = sc(0, 2)
                NBETA = sc(2, 2)
                Gv = sc(4, 2)
                t.act(BETA, ba.v((slice(None), slice(128, 130))), AF.Sigmoid)
                t.ts(NBETA, BETA, -1.0, None, ALU.mult)
                xa = sc(6, 2)
                t.tt(xa, ba.v((slice(None), slice(130, 132))), hp.v((slice(None), slice(2, 4))), ALU.add)
                ab = sc(8, 2)
                t.stt(ab, xa, -1.0, xa, ALU.mult, ALU.max)
                t.act(ab, ab, AF.Exp, scale=-1.0)
                t.ts(ab, ab, 1.0, None, ALU.add)
                t.act(ab, ab, AF.Ln)
                t.ts(xa, xa, 0.0, None, ALU.max)
                t.tt(xa, xa, ab, ALU.add)
                t.tt(Gv, xa, NAL.v(), ALU.mult)
                P = ps.next()
                t.mm(P.v((slice(None), slice(0, 2))), TRI, Gv)
                GC = sc(10, 2)
                NGC = sc(12, 2)
                EGC = sc(14, 2)
                t.cp(GC, P.v((slice(None), slice(0, 2))), eng="act")
                t.ts(NGC, GC, -1.0, None, ALU.mult)
                t.act(EGC, GC, AF.Exp)
                def dn_gen(hd):
                    KT = CV[2 + hd].v((slice(None), bs))
                    QT = CV[0 + hd].v((slice(None), bs))
                    VT = CV[4 + hd].v((slice(None), bs))
                    beta = sc(0 + hd)
                    nbeta = sc(2 + hd)
                    g = sc(4 + hd)
                    gc = sc(10 + hd)
                    ngc = sc(12 + hd)
                    egc = sc(14 + hd)
                    GL = sc(16 + hd)
                    EGL = sc(18 + hd)
                    EKD = sc(20 + hd)
                    GREP = roles.get(f"GREP{hd}", 1)
                    t.ts(GREP.v(), ONES, g, None, ALU.mult)
                    PG = ps.next()
                    pg = PG.v((slice(None), slice(0, 128)))
                    t.mm(pg, GREP.v(), TRI)
                    GBs = roles.get(f"GBs{hd}", 1)
                    t.cp(GBs.v(), pg, eng="act")
                    pg = GBs.v()
                    t.cp(GL, GBs.v((slice(None), slice(127, 128))))
                    t.act(EGL, GL, AF.Exp)
                    t.act(EKD, gc, AF.Exp, bias=GL, scale=-1.0)
                    yield
                    TMPD = roles.get(f"TMPD{hd}", 1)
                    t.tt(TMPD.v(), pg, NEGM, ALU.add)
                    DECT = roles.get(f"DECT{hd}", 1)
                    t.act(DECT.v(), TMPD.v(), AF.Exp, bias=ngc, scale=1.0)
                    EGB = roles.get(f"EGB{hd}", 1)
                    t.act(EGB.v(), pg, AF.Exp)
                    yield
                    PK = ps.next()
                    pk = PK.v((slice(None), slice(0, 128)))
                    t.mm(pk, KT, KT)
                    PQ = ps.next()
                    pq = PQ.v((slice(None), slice(0, 128)))
                    t.mm(pq, KT, QT)
                    NT = roles.get(f"NT{hd}", 1)
                    t.tt(NT.v(), pk, DECT.v(), ALU.mult)
                    t.stt(NT.v(), NT.v(), beta, STRICT, ALU.mult, ALU.mult)
                    AQKT = roles.get(f"AQKT{hd}", 1)
                    t.stt(AQKT.v(), pq, QSCALE, DECT.v(), ALU.mult, ALU.mult)
                    yield
                    PM = ps.next()
                    pm = PM.v((slice(None), slice(0, 128)))
                    t.tp(pm, NT.v(), IDENT)
                    M = roles.get(f"M{hd}", 1)
                    t.cp(M.v(), pm, eng="act")
                    PT_ = roles.get(f"PT_{hd}", 1)
                    t.tt(PT_.v(), IDENT, NT.v(), ALU.subtract)
                    yield
                    Nk, Mk = NT, M
                    for lvl in range(6):
                        PA = ps.next()
                        pa = PA.v((slice(None), slice(0, 128)))
                        t.mm(pa, Mk.v(), Nk.v())
                        PB = ps.next()
                        pb = PB.v((slice(None), slice(0, 128)))
                        t.mm(pb, Nk.v(), Mk.v())
                        N2 = roles.get(f"N2{hd}", 2)
                        M2 = roles.get(f"M2{hd}", 2)
                        t.cp(N2.v(), pa, eng="act")
                        t.cp(M2.v(), pb)
                        yield
                        PC = ps.next()
                        pc = PC.v((slice(None), slice(0, 128)))
                        t.mm(pc, M2.v(), PT_.v())
                        PN_ = roles.get(f"PN_{hd}", 2)
                        t.tt(PN_.v(), pc, PT_.v(), ALU.add)
                        yield
                        PT_ = PN_
                        Nk, Mk = N2, M2
                    TTm = PT_
                    PKT = ps.next()
                    pkt = PKT.v((slice(None), slice(0, 128)))
                    t.tp(pkt, KT, IDENT)
                    XK = roles.get(f"XK{hd}", 1)
                    KD = roles.get(f"KD{hd}", 1)
                    t.act(XK.v(), pkt, AF.Copy, scale=egc)
                    t.act(KD.v(), pkt, AF.Copy, scale=EKD)
                    yield
                    PVT = ps.next()
                    pvt = PVT.v((slice(None), slice(0, 128)))
                    t.tp(pvt, VT, IDENT)
                    XV = roles.get(f"XV{hd}", 1)
                    t.cp(XV.v(), pvt, eng="act")
                    yield
                    PW = ps.next()
                    pw = PW.v((slice(None), slice(0, 128)))
                    t.mm(pw, XK.v(), TTm.v())
                    WT = roles.get(f"WT{hd}", 1)
                    t.cp(WT.v(), pw, eng="act")
                    yield
                    PU = ps.next()
                    pu = PU.v((slice(None), slice(0, 128)))
                    t.mm(pu, TTm.v(), XV.v())
                    UP = roles.get(f"UP{hd}", 1)
                    t.ts(UP.v(), pu, beta, None, ALU.mult)
                    yield
                    QDT = roles.get(f"QDT{hd}", 1)
                    t.stt(QDT.v(), QT, QSCALE, EGB.v(), ALU.mult, ALU.mult)
                    P1 = ps.next()
                    p1 = P1.v((slice(None), slice(0, 128)))
                    t.mm(p1, WT.v(), S[hd].v())
                    VN = roles.get(f"VN{hd}", 1)
                    t.stt(VN.v(), p1, nbeta, UP.v(), ALU.mult, ALU.add)
                    yield
                    P2 = ps.next()
                    p2 = P2.v((slice(None), slice(0, 128)))
                    t.mm(p2, S[hd].v(), QDT.v(), start=True, stop=False)
                    t.mm(p2, VN.v(), AQKT.v(), start=False, stop=True)
                    P3 = ps.next()
                    p3 = P3.v((slice(None), slice(0, 128)))
                    t.mm(p3, KD.v(), VN.v())
                    t.stt(S[hd].v(), S[hd].v(), EGL, p3, ALU.mult, ALU.add)
                    OT = roles.get(f"OT{hd}", 1)
                    t.cp(OT.v(), p2, eng="act")
                    SQO = roles.get(f"SQO{hd}", 1)
                    t.act(SQO.v(), p2, AF.Square)
                    yield
                    PR = ps.next()
                    pr_ = PR.v((slice(None), slice(0, 128)))
                    t.mm(pr_, ONES128, SQO.v())
                    RS = roles.get(f"RS{hd}", 1)
                    t.act(RS.v(), pr_, AF.Ln, bias=eps6.v(), scale=1.0)
                    t.act(RS.v(), RS.v(), AF.Exp, scale=-0.5)
                    yield
                    t.stt(OT.v(), OT.v(), hp.v((slice(None), slice(6, 7))), RS.v(), ALU.mult, ALU.mult)
                    t.tt(outa[hd].v((slice(None), bs)), OT.v(), ZT[hd].v((slice(None), bs)), ALU.mult)
                svc = SV[bl]
                def swa_gen(h):
                    PS_ = ps.next()
                    L = Lr.next()
                    if nb > 0:
                        lo = 0
                    else:
                        lo = 128
                    n = 256 - lo
                    pv = PS_.v((slice(None), slice(0, n)))
                    t.mm(pv, SQT[h].v((slice(None), bs)),
                         SKT.v((slice(None), slice(bl * 128 + lo, bl * 128 + 256))))
                    lv = L.v((slice(None), slice(0, n)))
                    t.stt(lv, pv, QSCALE, alibi.v((slice(None), h, slice(lo, 256))), ALU.mult, ALU.add)
                    yield
                    mx = sc(22 + h)
                    nm = sc(24 + h)
                    t.rmax(mx, lv)
                    t.tt(mx, mx, hp.v((slice(None), slice(4 + h, 5 + h))), ALU.max)
                    t.ts(nm, mx, -1.0, None, ALU.mult)
                    rs = sc(22 + h)
                    es_ = sc(26 + h)
                    t.act(lv, lv, AF.Exp, bias=nm, scale=1.0)
                    t.op("dve", (lambda o, i: (lambda e: e.tensor_reduce(o, i, AX.X, ALU.add)))(rs.ap, lv.ap),
                         reads=[lv], writes=[rs])
                    t.act(es_, hp.v((slice(None), slice(4 + h, 5 + h))), AF.Exp, bias=nm, scale=1.0)
                    t.tt(rs, rs, es_, ALU.add)
                    t.op("dve", (lambda o, i: (lambda e: e.reciprocal(o, i)))(rs.ap, rs.ap), reads=[rs], writes=[rs])
                    t.ts(lv, lv, rs, None, ALU.mult)
                    yield
                    pts = []
                    for hf in range(n // 128):
                        PTP = ps.next()
                        ptp = PTP.v((slice(None), slice(0, 128)))
                        t.tp(ptp, L.v((slice(None), slice(hf * 128, (hf + 1) * 128))), IDENT)
                        pt = PTr.next()
                        t.cp(pt.v(), ptp, eng="act")
                        pts.append(pt)
                        yield
                    PO = ps.next()
                    po = PO.v((slice(None), slice(0, 128)))
                    if nb > 0:
                        t.mm(po, SVprev.v(), pts[0].v(), start=True, stop=False)
                        t.mm(po, svc.v(), pts[1].v(), start=False, stop=True)
                    else:
                        t.mm(po, svc.v(), pts[0].v(), start=True, stop=True)
                    t.cp(outb[h].v((slice(None), bs)), po, eng="act")
                gens = ([dn_gen(0), dn_gen(1)] if stage >= 3 else []) + [swa_gen(0), swa_gen(1)]
                while gens:
                    for gq in list(gens):
                        try:
                            next(gq)
                        except StopIteration:
                            gens.remove(gq)
                SVprev = svc
            t.cp(SKT.v((slice(None), slice(0, 128))), SKT.v((slice(None), slice(TT, TT + 128))))
            for h in range(2):
                t.dma("sp", V(outT.t[h * 128:(h + 1) * 128, t0:t0 + TT], outT.res), outa[h].v(), outa[h].v())
                t.dma("sp", V(outT.t[256 + h * 128:256 + (h + 1) * 128, t0:t0 + TT], outT.res), outb[h].v(), outb[h].v())
        t.emit()
    return nc


def _consts():
    i = np.arange(128)
    ident = np.eye(128, dtype=np.float32)
    ones = np.ones((128, 128), np.float32)
    ones128 = np.full((128, 128), 1.0 / 128, np.float32)
    tri = (i[:, None] <= i[None, :]).astype(np.float32)
    negm = np.where(i[:, None] <= i[None, :], 0.0, NEG).astype(np.float32)
    strict = (i[:, None] < i[None, :]).astype(np.float32)
    return np.ascontiguousarray(np.stack([ident, ones, ones128, tri, negm, strict], axis=1))


def _alibi(core):
    q = np.arange(128)[:, None]
    k = np.arange(256)[None, :]
    dist = q - k + 128
    valid = (dist >= 0) & (dist < 128)
    out = np.zeros((128, 2, 256), np.float32)
    for h in range(2):
        hq = 2 * core + h
        slope = np.float32(2.0) ** np.float32(-8.0 * (hq + 1) / 16)
        out[:, h, :] = np.where(valid, -slope * dist.astype(np.float32), np.float32(NEG))
    return out


def _pp(vec):
    return np.ascontiguousarray(vec.reshape(KC, 128).T)


_NC_CACHE = {}


def _get(kind, T):
    key = (kind, T)
    if key not in _NC_CACHE:
        if kind == "A":
            _NC_CACHE[key] = build_A(T)
        else:
            _NC_CACHE[key] = build_B(T, kind == "Bm")
    return _NC_CACHE[key]


def a_inputs(xT, w_in, conv_w, a_log, dt_bias, dn_norm_w, sinks, core):
    c = core
    cols = []
    for base in (0, 2048, 4096):
        for h in (2 * c, 2 * c + 1):
            cols.append(np.arange(base + h * 128, base + (h + 1) * 128))
    for h in (2 * c, 2 * c + 1):
        cols.append(np.arange(6144 + h * 128, 6144 + (h + 1) * 128))
    sq0 = 6144 + 2048 + 32
    for h in (2 * c, 2 * c + 1):
        cols.append(np.arange(sq0 + h * 128, sq0 + (h + 1) * 128))
    sk0 = sq0 + 2048
    kv = c // 2
    cols.append(np.arange(sk0 + kv * 128, sk0 + (kv + 1) * 128))
    wfm = np.ascontiguousarray(w_in[:, np.concatenate(cols)])
    b0 = 6144 + 2048
    a0 = b0 + 16
    sv0 = sk0 + 512
    tcols = np.concatenate([np.arange(sv0 + kv * 128, sv0 + (kv + 1) * 128),
                            [b0 + 2 * c, b0 + 2 * c + 1, a0 + 2 * c, a0 + 2 * c + 1]])
    wtm = np.zeros((w_in.shape[0], NTM), np.float32)
    wtm[:, :132] = w_in[:, tcols]
    convw = np.zeros((128, 6, 4), np.float32)
    ci = 0
    for base in (0, 2048, 4096):
        for h in (2 * c, 2 * c + 1):
            convw[:, ci, :] = conv_w[:, base + h * 128:base + (h + 1) * 128].T
            ci += 1
    hp = np.zeros((128, 8), np.float32)
    hp[:, 0:2] = a_log[2 * c:2 * c + 2][None, :]
    hp[:, 2:4] = dt_bias[2 * c:2 * c + 2][None, :]
    hp[:, 4:6] = sinks[2 * c:2 * c + 2][None, :]
    hp[:, 6] = dn_norm_w
    return {"xT": xT, "wfm": wfm, "wtm": wtm, "convw": convw, "hp": hp, "cst": _consts(), "alibi": _alibi(c)}


def run_A(xT, w_in, conv_w, a_log, dt_bias, dn_norm_w, sinks):
    T = xT.shape[1]
    nc = _get("A", T)
    in_maps = [a_inputs(xT, w_in, conv_w, a_log, dt_bias, dn_norm_w, sinks, c) for c in range(NCORE)]
    res = run_bass_kernel_spmd(nc, in_maps, core_ids=list(range(NCORE)))
    mixT = np.empty((D_MODEL, T), np.float32)
    for c in range(NCORE):
        o = res.results[c]["outT"]
        mixT[2 * c * 128:(2 * c + 2) * 128] = o[0:256]
        mixT[2048 + 2 * c * 128:2048 + (2 * c + 2) * 128] = o[256:512]
    return mixT


NB = 4


def run_B(xT, mixT, w_o, g1, b1, g2, b2, ffn, moe):
    T = xT.shape[1]
    Tc = T // NB
    nc = _get("Bm" if moe else "Bd", Tc)
    lnp = np.ascontiguousarray(np.stack([_pp(g1), _pp(b1), _pp(g2), _pp(b2)], axis=1))
    ones = np.full((128, 128), 1.0 / D_MODEL, np.float32)
    ident = np.eye(128, dtype=np.float32)
    base = {"w_o": w_o, "lnp": lnp, "onesd": ones, "ident": ident}
    if moe:
        rw, ew1, ew3, ew2 = ffn
        base.update({"rw": np.ascontiguousarray(rw.reshape(KC, 128, 8).transpose(1, 0, 2)), "w1": ew1, "w3": ew3, "w2": ew2})
    else:
        fw1, fw3, fw2 = ffn
        base.update({"w1": np.ascontiguousarray(fw1.reshape(D_MODEL, 4, 2048).transpose(1, 0, 2)),
                     "w3": np.ascontiguousarray(fw3.reshape(D_MODEL, 4, 2048).transpose(1, 0, 2)),
                     "w2": fw2.reshape(4, 2048, D_MODEL)})
    in_maps = []
    for c in range(NB):
        m = dict(base)
        m["xT"] = np.ascontiguousarray(xT[:, c * Tc:(c + 1) * Tc])
        m["mixT"] = np.ascontiguousarray(mixT[:, c * Tc:(c + 1) * Tc])
        in_maps.append(m)
    res = run_bass_kernel_spmd(nc, in_maps, core_ids=list(range(NB)))
    return np.concatenate([res.results[c]["yT"] for c in range(NB)], axis=1)


def kernel(x, w_in, conv_w, a_log, dt_bias, dn_norm_w, sinks, w_o, ln1_g, ln1_b,
           ffn_w1, ffn_w3, ffn_w2, router_w, exp_w1, exp_w3, exp_w2, ln2_g, ln2_b):
    f = lambda a: np.asarray(a, dtype=np.float32)
    xT = np.ascontiguousarray(f(x)[0].T)
    for i in range(DEPTH):
        mixT = run_A(xT, f(w_in[i]), f(conv_w[i]), f(a_log[i]), f(dt_bias[i]), f(dn_norm_w[i]), f(sinks[i]))
        j = i // 2
        if i % 2 == 0:
            ffn = (f(ffn_w1[j]), f(ffn_w3[j]), f(ffn_w2[j]))
        else:
            ffn = (f(router_w[j]), f(exp_w1[j]), f(exp_w3[j]), f(exp_w2[j]))
        xT = run_B(xT, mixT, f(w_o[i]), f(ln1_g[i]), f(ln1_b[i]), f(ln2_g[i]), f(ln2_b[i]), ffn, i % 2 == 1)
    return np.ascontiguousarray(xT.T)[None].astype(np.float32)
```

```python
import numpy as np
from contextlib import ExitStack
import concourse.bass as bass
import concourse.mybir as mybir
from concourse.bass_utils import run_bass_kernel_spmd

F32 = mybir.dt.float32
BF16 = mybir.dt.bfloat16
AF = mybir.ActivationFunctionType
ALU = mybir.AluOpType
AX = mybir.AxisListType

D_MODEL = 4096
SEQ = 16384
DEPTH = 4
NCORE = 8
KC = D_MODEL // 128
DN_ALPHA = (2 * DEPTH) ** 0.25
LN_EPS = 1e-5
NEG = -1e30
QSCALE = 128 ** -0.5


class Res:
    __slots__ = ("name", "lw", "rd", "sem", "cnt")

    def __init__(self, name):
        self.name = name
        self.lw = None
        self.rd = []
        self.sem = None
        self.cnt = 0


class V:
    __slots__ = ("ap", "res")

    def __init__(self, ap, res):
        self.ap = ap
        self.res = res


class Buf:
    def __init__(self, name, t, nres=1):
        self.name = name
        self.t = t
        self.res = [Res(f"{name}.{i}") for i in range(nres)]

    def v(self, idx=None, r=None):
        ap = self.t[:] if idx is None else self.t[idx]
        if r is None:
            rs = self.res
        elif isinstance(r, int):
            rs = [self.res[r]]
        else:
            rs = [self.res[i] for i in r]
        return V(ap, rs)


class Op:
    __slots__ = ("id", "eng", "fn", "deps", "dma", "semres", "tok", "inc", "ms")


EPOCH = 20000


class TR:
    ENG = ("pe", "act", "dve", "pool", "sp")

    def __init__(self, nc, es):
        self.nc = nc
        self.es = es
        self.ops = []
        self.nbuf = 0

    def sbuf(self, name, shape, dt, nres=1):
        t = self.es.enter_context(self.nc.sbuf_tensor(name, list(shape), dt))
        return Buf(name, t, nres)

    def psum(self, name, shape, dt):
        t = self.es.enter_context(self.nc.psum_tensor(name, list(shape), dt))
        return Buf(name, t, 1)

    def dram(self, name, shape, dt, kind):
        t = self.nc.dram_tensor(name, list(shape), dt, kind=kind).ap()
        return Buf(name, t, 1)

    def op(self, eng, fn, reads=(), writes=(), dma=False, semres=None):
        o = Op()
        o.id = len(self.ops)
        o.eng = eng
        o.fn = fn
        o.dma = dma
        o.semres = semres
        o.inc = False
        o.ms = None
        o.tok = None
        deps = {}
        rres = []
        for v in reads:
            rres.extend(v.res)
        wres = []
        for v in writes:
            wres.extend(v.res)
        for r in rres:
            if r.lw is not None:
                deps[r.lw] = "raw"
        for w in wres:
            if w.lw is not None and w.lw not in deps:
                deps[w.lw] = "waw"
            for x in w.rd:
                if x not in deps:
                    deps[x] = "war"
        for r in rres:
            r.rd.append(o.id)
        for w in wres:
            w.lw = o.id
            w.rd = []
        keep = []
        for d, kind in deps.items():
            p = self.ops[d]
            if (not p.dma) and (not dma) and p.eng == eng:
                if eng == "pe":
                    continue
                if kind != "raw":
                    continue
            keep.append(d)
        o.deps = sorted(keep)
        self.ops.append(o)
        return o

    def dma(self, q, out, in_, semside):
        o_ap, i_ap = out.ap, in_.ap
        return self.op(q, lambda e, s, n: e.dma_start(out=o_ap, in_=i_ap).then_inc(s, 16),
                       reads=[in_], writes=[out], dma=True, semres=semside.res[0])

    def emit(self):
        nc = self.nc
        ops = self.ops
        for o in ops:
            if o.dma:
                r = o.semres
                if r.sem is None:
                    r.sem = self.es.enter_context(nc.semaphore(f"d{o.id}"))
                r.cnt += 16
                o.tok = (r.sem, r.cnt)
        waited = {e: {} for e in self.ENG}
        for o in ops:
            w = waited[o.eng]
            for d in o.deps:
                p = ops[d]
                if p.dma:
                    continue
                if w.get(p.eng, -1) < p.id:
                    p.inc = True
                    w[p.eng] = p.id
        cnt = {e: 0 for e in self.ENG}
        esems = {}
        for o in ops:
            if (not o.dma) and o.inc:
                k = cnt[o.eng]
                cnt[o.eng] += 1
                ep = k // EPOCH
                key = (o.eng, ep)
                if key not in esems:
                    esems[key] = self.es.enter_context(nc.semaphore(f"e_{o.eng}_{ep}"))
                o.tok = (esems[key], k % EPOCH + 1)
        streams = {e: [] for e in self.ENG}
        waited = {e: {} for e in self.ENG}
        for o in ops:
            w = waited[o.eng]
            st = streams[o.eng]
            for d in o.deps:
                p = ops[d]
                if p.tok is None:
                    continue
                sem, val = p.tok
                key = id(sem)
                if w.get(key, 0) < val:
                    w[key] = val
                    st.append(("w", sem, val))
            st.append(("o", o))
        seen = {}
        for o in ops:
            if o.dma:
                seen[id(o.semres.sem)] = (o.semres.sem, o.semres.cnt)
        for sem, val in seen.values():
            streams["sp"].append(("w", sem, val))

        def run(eng, st):
            for it in st:
                if it[0] == "w":
                    eng.wait_ge(it[1], it[2])
                else:
                    o = it[1]
                    if o.dma:
                        o.fn(eng, o.tok[0], 16)
                    else:
                        ins = o.fn(eng)
                        if o.inc:
                            ins.then_inc(o.tok[0], 1)

        with nc.Block() as block:
            @block.tensor
            def _(e):
                run(e, streams["pe"])

            @block.scalar
            def _(e):
                run(e, streams["act"])

            @block.vector
            def _(e):
                run(e, streams["dve"])

            @block.gpsimd
            def _(e):
                run(e, streams["pool"])

            @block.sync
            def _(e):
                run(e, streams["sp"])

    def mm(self, out, lhsT, rhs, start=True, stop=True):
        o, l, r = out.ap, lhsT.ap, rhs.ap
        return self.op("pe", lambda e: e.matmul(o, l, r, start=start, stop=stop),
                       reads=[lhsT, rhs], writes=[out])

    def tp(self, out, in_, ident):
        o, i, d = out.ap, in_.ap, ident.ap
        return self.op("pe", lambda e: e.transpose(o, i, d), reads=[in_, ident], writes=[out])

    def act(self, out, in_, func, bias=None, scale=None, accum=None, eng="act"):
        o, i = out.ap, in_.ap
        kw = {}
        reads = [in_]
        writes = [out]
        if bias is not None:
            if isinstance(bias, V):
                kw["bias"] = bias.ap
                reads.append(bias)
            else:
                kw["bias"] = bias
        if scale is not None:
            if isinstance(scale, V):
                kw["scale"] = scale.ap
                reads.append(scale)
            else:
                kw["scale"] = scale
        if accum is not None:
            kw["accum_out"] = accum.ap
            writes.append(accum)
        return self.op(eng, lambda e: e.activation(o, i, func, **kw), reads=reads, writes=writes)

    def ts(self, out, in0, s1, s2, op0, op1=None, eng="dve"):
        o, i = out.ap, in0.ap
        reads = [in0]
        a1 = s1
        a2 = s2
        if isinstance(s1, V):
            a1 = s1.ap
            reads.append(s1)
        if isinstance(s2, V):
            a2 = s2.ap
            reads.append(s2)
        if op1 is None:
            return self.op(eng, lambda e: e.tensor_scalar(o, i, a1, None, op0), reads=reads, writes=[out])
        return self.op(eng, lambda e: e.tensor_scalar(o, i, a1, a2, op0, op1), reads=reads, writes=[out])

    def stt(self, out, in0, s, in1, op0, op1, eng="dve"):
        o, i0, i1 = out.ap, in0.ap, in1.ap
        reads = [in0, in1]
        a = s
        if isinstance(s, V):
            a = s.ap
            reads.append(s)
        return self.op(eng, lambda e: e.scalar_tensor_tensor(o, i0, a, i1, op0, op1), reads=reads, writes=[out])

    def tt(self, out, in0, in1, op, eng="dve"):
        o, i0, i1 = out.ap, in0.ap, in1.ap
        return self.op(eng, lambda e: e.tensor_tensor(o, i0, i1, op), reads=[in0, in1], writes=[out])

    def cp(self, out, in_, eng="dve"):
        o, i = out.ap, in_.ap
        if eng == "act":
            return self.op(eng, lambda e: e.copy(o, i), reads=[in_], writes=[out])
        return self.op(eng, lambda e: e.tensor_copy(o, i), reads=[in_], writes=[out])

    def memset(self, out, val, eng="dve"):
        o = out.ap
        return self.op(eng, lambda e: e.memset(o, val), reads=[], writes=[out])

    def rmax(self, out, in_):
        o, i = out.ap, in_.ap
        return self.op("dve", lambda e: e.tensor_reduce(o, i, AX.X, ALU.max), reads=[in_], writes=[out])


class Rot:
    def __init__(self, bufs):
        self.bufs = bufs
        self.i = 0

    def next(self):
        b = self.bufs[self.i % len(self.bufs)]
        self.i += 1
        return b


def emit_ln_fm(t, X, N, ps, ones, lnp, gi, bi, sqrot, meanB, rstdB, tmpB, epsv, post=None):
    S = ps.next()
    Q = ps.next()
    for kc in range(KC):
        t.mm(S.v((slice(None), slice(0, N))), ones.v(), X.v((slice(None), kc, slice(None)), kc),
             start=(kc == 0), stop=(kc == KC - 1))
    for kc in range(KC):
        sq = sqrot.next()
        t.act(sq.v(), X.v((slice(None), kc, slice(None)), kc), AF.Square)
        t.mm(Q.v((slice(None), slice(0, N))), ones.v(), sq.v(), start=(kc == 0), stop=(kc == KC - 1))
    t.cp(meanB.v(), S.v((slice(None), slice(0, N))), eng="act")
    t.tt(tmpB.v(), meanB.v(), meanB.v(), ALU.mult)
    t.tt(tmpB.v(), Q.v((slice(None), slice(0, N))), tmpB.v(), ALU.subtract)
    t.act(rstdB.v(), tmpB.v(), AF.Ln, bias=epsv, scale=1.0)
    t.act(rstdB.v(), rstdB.v(), AF.Exp, scale=-0.5)
    for kc in range(KC):
        xv = X.v((slice(None), kc, slice(None)), kc)
        t.tt(xv, xv, meanB.v(), ALU.subtract)
        t.tt(xv, xv, rstdB.v(), ALU.mult)
        t.ts(xv, xv, lnp.v((slice(None), gi, slice(kc, kc + 1))), lnp.v((slice(None), bi, slice(kc, kc + 1))),
             ALU.mult, ALU.add)
        if post is not None:
            post(kc, xv)


def build_B(T, moe):
    TG = 512
    NE = 8 if moe else 4
    nc = bass.Bass("TRN2", target_bir_lowering=False)
    es = ExitStack()
    with es:
        t = TR(nc, es)
        xT = t.dram("xT", [D_MODEL, T], F32, "ExternalInput")
        mixT = t.dram("mixT", [D_MODEL, T], F32, "ExternalInput")
        w_o = t.dram("w_o", [D_MODEL, D_MODEL], F32, "ExternalInput")
        lnp_d = t.dram("lnp", [128, 4, KC], F32, "ExternalInput")
        ones_d = t.dram("onesd", [128, 128], F32, "ExternalInput")
        ident_d = t.dram("ident", [128, 128], F32, "ExternalInput")
        w1 = t.dram("w1", [NE, D_MODEL, 2048], F32, "ExternalInput")
        w3 = t.dram("w3", [NE, D_MODEL, 2048], F32, "ExternalInput")
        w2 = t.dram("w2", [NE, 2048, D_MODEL], F32, "ExternalInput")
        if moe:
            rw_d = t.dram("rw", [128, KC, 8], F32, "ExternalInput")
        yT = t.dram("yT", [D_MODEL, T], F32, "ExternalOutput")

        X = t.sbuf("X", [128, KC, TG], F32, nres=KC)
        A = t.sbuf("A", [128, KC, TG], BF16, nres=KC)
        WS = Rot([t.sbuf(f"WS{i}", [128, 8192], BF16) for i in range(3 if moe else 4)])
        H = t.sbuf("H", [128, 16, TG], BF16, nres=16)
        lnp = t.sbuf("lnp_s", [128, 4, KC], F32)
        ones = t.sbuf("ones_s", [128, 128], F32)
        ident = t.sbuf("ident_s", [128, 128], F32)
        sqrot = Rot([t.sbuf(f"sq{i}", [128, TG], F32) for i in range(2)])
        meanB = t.sbuf("meanB", [128, TG], F32)
        rstdB = t.sbuf("rstdB", [128, TG], F32)
        tmpB = t.sbuf("tmpB", [128, TG], F32)
        sgrot = Rot([t.sbuf(f"sg{i}", [128, TG], F32) for i in range(2)])
        ps = Rot([t.psum(f"ps{i}", [128, 512], F32) for i in range(8)])
        if moe:
            RW = t.sbuf("RW", [128, KC, 8], F32)
            GATE = t.sbuf("GATE", [128, 8, TG], F32, nres=8)
            LG = t.sbuf("LG", [128, 8], F32)
            L2 = t.sbuf("L2", [128, 8], F32)
            EQ = t.sbuf("EQ", [128, 8], F32)
            GT = t.sbuf("GT", [128, 8], F32)
            sm = t.sbuf("sm", [128, 8], F32, nres=8)
            REP = Rot([t.sbuf(f"rep{i}", [128, 128], F32) for i in range(2)])
            onesfull = t.sbuf("onesfull", [128, 128], F32)
            t.dma("sp", RW.v(), rw_d.v(), RW.v())
            t.memset(onesfull.v(), 1.0)

        epsb = t.sbuf("epsb", [128, 1], F32)
        t.memset(epsb.v(), LN_EPS)
        t.dma("sp", lnp.v(), lnp_d.v(), lnp.v())
        t.dma("sp", ones.v(), ones_d.v(), ones.v())
        t.dma("sp", ident.v(), ident_d.v(), ident.v())

        xT3 = xT.t.rearrange("(kc p) t -> p kc t", p=128)
        mixT3 = mixT.t.rearrange("(kc p) t -> p kc t", p=128)
        yT3 = yT.t.rearrange("(kc p) t -> p kc t", p=128)
        wo3 = w_o.t.rearrange("(kc p) n -> p kc n", p=128)

        for g in range(T // TG):
            t0 = g * TG
            for half in range(2):
                ks = slice(half * 16, half * 16 + 16)
                t.dma("sp", V(X.t[:, ks, :], X.res[half * 16:half * 16 + 16]),
                      V(xT3[:, ks, t0:t0 + TG], xT.res), V(X.t[:, ks, :], [X.res[half * 16]]))
                t.dma("pool", V(A.t[:, ks, :], A.res[half * 16:half * 16 + 16]),
                      V(mixT3[:, ks, t0:t0 + TG], mixT.res), V(A.t[:, ks, :], [A.res[half * 16]]))
            for ds in range(D_MODEL // 256):
                W = WS.next()
                Wv = W.t[:, :].rearrange("p (kc n) -> p kc n", kc=KC)
                t.dma("pool", V(Wv, W.res), V(wo3[:, :, ds * 256:(ds + 1) * 256], w_o.res), W.v())
                for j in range(2):
                    dt_ = ds * 2 + j
                    P = ps.next()
                    for kc in range(KC):
                        t.mm(P.v(), V(Wv[:, kc, j * 128:(j + 1) * 128], W.res),
                             A.v((slice(None), kc, slice(None)), kc), start=(kc == 0), stop=(kc == KC - 1))
                    xv = X.v((slice(None), dt_, slice(None)), dt_)
                    t.stt(xv, xv, DN_ALPHA, P.v(), ALU.mult, ALU.add)
            def post1(kc, xv):
                t.cp(A.v((slice(None), kc, slice(None)), kc), xv, eng="act")
            emit_ln_fm(t, X, TG, ps, ones, lnp, 0, 1, sqrot, meanB, rstdB, tmpB, epsb.v(), post=post1)
            if moe:
                for j in range(TG // 128):
                    R = ps.next()
                    for kc in range(KC):
                        t.mm(R.v((slice(None), slice(0, 8))), X.v((slice(None), kc, slice(j * 128, (j + 1) * 128)), kc),
                             RW.v((slice(None), kc, slice(None))), start=(kc == 0), stop=(kc == KC - 1))
                    t.cp(LG.v(), R.v((slice(None), slice(0, 8))))
                    m1 = sm.v((slice(None), slice(0, 1)), 0)
                    m2 = sm.v((slice(None), slice(1, 2)), 1)
                    nm1 = sm.v((slice(None), slice(2, 3)), 2)
                    ssum = sm.v((slice(None), slice(3, 4)), 3)
                    rs = sm.v((slice(None), slice(4, 5)), 4)
                    t.rmax(m1, LG.v())
                    t.ts(EQ.v(), LG.v(), m1, NEG, ALU.is_equal, ALU.mult)
                    t.tt(L2.v(), LG.v(), EQ.v(), ALU.add)
                    t.rmax(m2, L2.v())
                    t.ts(EQ.v(), LG.v(), m2, None, ALU.is_ge)
                    t.ts(nm1, m1, -1.0, None, ALU.mult)
                    t.act(L2.v(), LG.v(), AF.Exp, bias=nm1, scale=1.0)
                    t.tt(L2.v(), L2.v(), EQ.v(), ALU.mult)
                    t.op("dve", (lambda o, i: (lambda e: e.tensor_reduce(o, i, AX.X, ALU.add)))(ssum.ap, L2.t[:]),
                         reads=[L2.v()], writes=[ssum])
                    t.op("dve", (lambda o, i: (lambda e: e.reciprocal(o, i)))(rs.ap, ssum.ap), reads=[ssum], writes=[rs])
                    t.ts(GT.v(), L2.v(), rs, None, ALU.mult)
                    for e_ in range(8):
                        rp = REP.next()
                        t.ts(rp.v(), onesfull.v(), GT.v((slice(None), slice(e_, e_ + 1))), None, ALU.mult)
                        GP = ps.next()
                        t.mm(GP.v((slice(None), slice(0, 128))), rp.v(), ident.v())
                        t.cp(GATE.v((slice(None), e_, slice(j * 128, (j + 1) * 128)), e_),
                             GP.v((slice(None), slice(0, 128))), eng="act")
            for kc in range(KC):
                xv = X.v((slice(None), kc, slice(None)), kc)
                t.act(xv, xv, AF.Copy, scale=DN_ALPHA)
            for e_ in range(NE):
                w13 = [w1.t[e_].rearrange("(kc p) n -> p kc n", p=128), w3.t[e_].rearrange("(kc p) n -> p kc n", p=128)]
                w2e = w2.t[e_].rearrange("(c p) n -> p c n", p=128)
                for s in range(8):
                    Wb = []
                    for wi in range(2):
                        W = WS.next()
                        Wv = W.t[:, :].rearrange("p (kc n) -> p kc n", kc=KC)
                        t.dma("pool", V(Wv, W.res), V(w13[wi][:, :, s * 256:(s + 1) * 256], w1.res), W.v())
                        Wb.append((W, Wv))
                    for c in range(2):
                        P1 = ps.next()
                        P3 = ps.next()
                        for kc in range(KC):
                            t.mm(P1.v(), V(Wb[0][1][:, kc, c * 128:(c + 1) * 128], Wb[0][0].res),
                                 A.v((slice(None), kc, slice(None)), kc), start=(kc == 0), stop=(kc == KC - 1))
                        for kc in range(KC):
                            t.mm(P3.v(), V(Wb[1][1][:, kc, c * 128:(c + 1) * 128], Wb[1][0].res),
                                 A.v((slice(None), kc, slice(None)), kc), start=(kc == 0), stop=(kc == KC - 1))
                        sg = sgrot.next()
                        t.act(sg.v(), P1.v(), AF.Silu)
                        if moe:
                            t.tt(sg.v(), sg.v(), GATE.v((slice(None), e_, slice(None)), e_), ALU.mult)
                        hc = s * 2 + c
                        t.tt(H.v((slice(None), hc, slice(None)), hc), sg.v(), P3.v(), ALU.mult)
                for ds in range(8):
                    W = WS.next()
                    Wv = W.t[:, :].rearrange("p (c n) -> p c n", c=16)
                    t.dma("pool", V(Wv, W.res), V(w2e[:, :, ds * 512:(ds + 1) * 512], w2.res), W.v())
                    for j in range(4):
                        dt_ = ds * 4 + j
                        P = ps.next()
                        for c in range(16):
                            t.mm(P.v(), V(Wv[:, c, j * 128:(j + 1) * 128], W.res),
                                 H.v((slice(None), c, slice(None)), c), start=(c == 0), stop=(c == 15))
                        xv = X.v((slice(None), dt_, slice(None)), dt_)
                        t.tt(xv, xv, P.v(), ALU.add)
            emit_ln_fm(t, X, TG, ps, ones, lnp, 2, 3, sqrot, meanB, rstdB, tmpB, epsb.v())
            for half in range(2):
                ks = slice(half * 16, half * 16 + 16)
                t.dma("sp", V(yT3[:, ks, t0:t0 + TG], yT.res), V(X.t[:, ks, :], X.res[half * 16:half * 16 + 16]),
                      V(X.t[:, ks, :], [X.res[half * 16]]))
        t.emit()
    return nc


NFM = 11
NTM = 256


def build_A(T, stage=3):
    TT = 256
    nc = bass.Bass("TRN2", target_bir_lowering=False)
    es = ExitStack()
    with es:
        t = TR(nc, es)
        xT = t.dram("xT", [D_MODEL, T], F32, "ExternalInput")
        wfm_d = t.dram("wfm", [D_MODEL, NFM * 128], F32, "ExternalInput")
        wtm_d = t.dram("wtm", [D_MODEL, NTM], F32, "ExternalInput")
        convw_d = t.dram("convw", [128, 6, 4], F32, "ExternalInput")
        hp_d = t.dram("hp", [128, 8], F32, "ExternalInput")
        cst_d = t.dram("cst", [128, 6, 128], F32, "ExternalInput")
        alibi_d = t.dram("alibi", [128, 2, 256], F32, "ExternalInput")
        outT = t.dram("outT", [512, T], F32, "ExternalOutput")

        WFM = t.sbuf("WFM", [128, KC, NFM * 128], BF16)
        WTM = t.sbuf("WTM", [128, KC, NTM], BF16)
        XB = Rot([t.sbuf(f"XB{i}", [128, KC, TT], BF16) for i in range(2)])
        convw = t.sbuf("convw_s", [128, 6, 4], F32)
        hp = t.sbuf("hp_s", [128, 8], F32)
        cst = t.sbuf("cst_s", [128, 6, 128], F32)
        alibi = t.sbuf("alibi_s", [128, 2, 256], F32)
        ps = Rot([t.psum(f"ps{i}", [128, 512], F32) for i in range(8)])

        def c_(i):
            return cst.v((slice(None), i, slice(None)))
        IDENT, ONES, ONES128, TRI, NEGM, STRICT = [c_(i) for i in range(6)]

        PRE = [t.sbuf(f"PRE{i}", [128, 3 + TT], F32) for i in range(6)]
        CV = [t.sbuf(f"CV{i}", [128, TT], F32) for i in range(6)]
        ZT = [t.sbuf(f"ZT{i}", [128, TT], F32) for i in range(2)]
        SQT = [t.sbuf(f"SQT{i}", [128, TT], BF16) for i in range(2)]
        SKT = t.sbuf("SKT", [128, 128 + TT], BF16)
        SVr = Rot([t.sbuf(f"SV{i}", [128, 128], BF16) for i in range(3)])
        BAr = Rot([t.sbuf(f"BA{i}", [128, 132], F32) for i in range(2)])
        cacc = t.sbuf("cacc", [128, TT], F32)
        tmpA = Rot([t.sbuf(f"tmpA{i}", [128, TT], F32) for i in range(2)])
        smr = Rot([t.sbuf(f"smA{i}", [128, 28], F32, nres=28) for i in range(2)])
        S = [t.sbuf(f"S{i}", [128, 128], F32) for i in range(2)]
        class Roles:
            def __init__(self):
                self.d = {}

            def get(self, name, n=2):
                if name not in self.d:
                    self.d[name] = Rot([t.sbuf(f"r_{name}{i}", [128, 128], F32) for i in range(n)])
                return self.d[name].next()
        roles = Roles()
        OUTA = [Rot([t.sbuf(f"OUTA{h}_{i}", [128, TT], F32) for i in range(2)]) for h in range(2)]
        OUTB = [Rot([t.sbuf(f"OUTB{h}_{i}", [128, TT], F32) for i in range(2)]) for h in range(2)]
        Lr = Rot([t.sbuf(f"L{i}", [128, 256], F32) for i in range(2)])
        PTr = Rot([t.sbuf(f"PT{i}", [128, 128], BF16) for i in range(4)])
        NAL = t.sbuf("NAL", [128, 2], F32)
        eps6 = t.sbuf("eps6", [128, 1], F32)
        t.memset(eps6.v(), 1e-6)

        for (sb, dr) in ((convw, convw_d), (hp, hp_d), (cst, cst_d), (alibi, alibi_d)):
            t.dma("sp", sb.v(), dr.v(), sb.v())
        wfm3 = wfm_d.t.rearrange("(kc p) n -> p kc n", p=128)
        wtm3 = wtm_d.t.rearrange("(kc p) n -> p kc n", p=128)
        for q4 in range(4):
            ks = slice(q4 * 8, q4 * 8 + 8)
            t.dma("pool", V(WFM.t[:, ks, :], WFM.res), V(wfm3[:, ks, :], wfm_d.res), WFM.v())
        t.dma("pool", WTM.v(), V(wtm3, wtm_d.res), WTM.v())
        for i in range(6):
            t.memset(PRE[i].v((slice(None), slice(0, 3))), 0.0)
        for i in range(2):
            t.memset(S[i].v(), 0.0)
        t.act(NAL.v(), hp.v((slice(None), slice(0, 2))), AF.Exp)
        t.ts(NAL.v(), NAL.v(), -1.0, None, ALU.mult)

        xT3 = xT.t.rearrange("(kc p) t -> p kc t", p=128)
        SVprev = None
        for it in range(T // TT):
            t0 = it * TT
            xb = XB.next()
            for half in range(2):
                ks = slice(half * 16, half * 16 + 16)
                t.dma("pool", V(xb.t[:, ks, :], xb.res), V(xT3[:, ks, t0:t0 + TT], xT.res), xb.v())
            for ci in range(NFM):
                P = ps.next()
                pv = P.v((slice(None), slice(0, TT)))
                for kc in range(KC):
                    t.mm(pv, WFM.v((slice(None), kc, slice(ci * 128, (ci + 1) * 128))),
                         xb.v((slice(None), kc, slice(None))), start=(kc == 0), stop=(kc == KC - 1))
                if ci < 6:
                    t.cp(PRE[ci].v((slice(None), slice(3, 3 + TT))), pv, eng="act")
                elif ci < 8:
                    t.act(ZT[ci - 6].v(), pv, AF.Silu)
                elif ci < 10:
                    t.cp(SQT[ci - 8].v(), pv, eng="act")
                else:
                    t.cp(SKT.v((slice(None), slice(128, 128 + TT))), pv, eng="act")
            BA = []
            SV = []
            for bl in range(TT // 128 if stage >= 0.5 else 0):
                P = ps.next()
                pv = P.v((slice(None), slice(0, NTM)))
                for kc in range(KC):
                    t.mm(pv, xb.v((slice(None), kc, slice(bl * 128, (bl + 1) * 128))),
                         WTM.v((slice(None), kc, slice(None))), start=(kc == 0), stop=(kc == KC - 1))
                ba = BAr.next()
                sv = SVr.next()
                t.cp(ba.v(), P.v((slice(None), slice(0, 132))), eng="act")
                t.cp(sv.v(), ba.v((slice(None), slice(0, 128))), eng="act")
                BA.append(ba)
                SV.append(sv)
            for ci in range(6 if stage >= 1 else 0):
                pr = PRE[ci]
                t.ts(cacc.v(), pr.v((slice(None), slice(0, TT))), convw.v((slice(None), ci, slice(0, 1))), None, ALU.mult)
                for k in range(1, 4):
                    t.stt(cacc.v(), pr.v((slice(None), slice(k, k + TT))), convw.v((slice(None), ci, slice(k, k + 1))),
                          cacc.v(), ALU.mult, ALU.add)
                t.act(CV[ci].v(), cacc.v(), AF.Silu)
                t.cp(pr.v((slice(None), slice(0, 3))), pr.v((slice(None), slice(TT, TT + 3))))
                if ci < 4:
                    tq = tmpA.next()
                    t.act(tq.v(), CV[ci].v(), AF.Square)
                    P = ps.next()
                    pv = P.v((slice(None), slice(0, TT)))
                    t.mm(pv, ONES, tq.v())
                    t.act(tq.v(), pv, AF.Ln, bias=eps6.v(), scale=1.0)
                    t.act(tq.v(), tq.v(), AF.Exp, scale=-0.5)
                    t.tt(CV[ci].v(), CV[ci].v(), tq.v(), ALU.mult)
            outa = [OUTA[h].next() for h in range(2)]
            outb = [OUTB[h].next() for h in range(2)]
            if stage <= 1:
                for h in range(2):
                    if stage == 1:
                        t.cp(outa[h].v(), CV[h].v())
                        t.cp(outb[h].v(), CV[2 + h].v())
                    else:
                        t.cp(outa[h].v(), PRE[h].v((slice(None), slice(3, 3 + TT))))
                        t.cp(outb[h].v(), ZT[h].v())
            for bl in range(TT // 128 if stage >= 2 else 0):
                nb = (t0 // 128) + bl
                bs = slice(bl * 128, (bl + 1) * 128)
                sm = smr.next()

                def sc(i, n=1):
                    return sm.v((slice(None), slice(i, i + n)), list(range(i, i + n)))
                ba = BA[bl]
                BETA = sc(0, 2)
                NBETA = sc(2, 2)
                Gv = sc(4, 2)
                t.act(BETA, ba.v((slice(None), slice(128, 130))), AF.Sigmoid)
                t.ts(NBETA, BETA, -1.0, None, ALU.mult)
                xa = sc(6, 2)
                t.tt(xa, ba.v((slice(None), slice(130, 132))), hp.v((slice(None), slice(2, 4))), ALU.add)
                ab = sc(8, 2)
                t.stt(ab, xa, -1.0, xa, ALU.mult, ALU.max)
                t.act(ab, ab, AF.Exp, scale=-1.0)
                t.ts(ab, ab, 1.0, None, ALU.add)
                t.act(ab, ab, AF.Ln)
                t.ts(xa, xa, 0.0, None, ALU.max)
                t.tt(xa, xa, ab, ALU.add)
                t.tt(Gv, xa, NAL.v(), ALU.mult)
                P = ps.next()
                t.mm(P.v((slice(None), slice(0, 2))), TRI, Gv)
                GC = sc(10, 2)
                NGC = sc(12, 2)
                EGC = sc(14, 2)
                t.cp(GC, P.v((slice(None), slice(0, 2))), eng="act")
                t.ts(NGC, GC, -1.0, None, ALU.mult)
                t.act(EGC, GC, AF.Exp)
                def dn_gen(hd):
                    KT = CV[2 + hd].v((slice(None), bs))
                    QT = CV[0 + hd].v((slice(None), bs))
                    VT = CV[4 + hd].v((slice(None), bs))
                    beta = sc(0 + hd)
                    nbeta = sc(2 + hd)
                    g = sc(4 + hd)
                    gc = sc(10 + hd)
                    ngc = sc(12 + hd)
                    egc = sc(14 + hd)
                    GL = sc(16 + hd)
                    EGL = sc(18 + hd)
                    EKD = sc(20 + hd)
                    GREP = roles.get(f"GREP{hd}", 1)
                    t.ts(GREP.v(), ONES, g, None, ALU.mult)
                    PG = ps.next()
                    pg = PG.v((slice(None), slice(0, 128)))
                    t.mm(pg, GREP.v(), TRI)
                    GBs = roles.get(f"GBs{hd}", 1)
                    t.cp(GBs.v(), pg, eng="act")
                    pg = GBs.v()
                    t.cp(GL, GBs.v((slice(None), slice(127, 128))))
                    t.act(EGL, GL, AF.Exp)
                    t.act(EKD, gc, AF.Exp, bias=GL, scale=-1.0)
                    yield
                    TMPD = roles.get(f"TMPD{hd}", 1)
                    t.tt(TMPD.v(), pg, NEGM, ALU.add)
                    DECT = roles.get(f"DECT{hd}", 1)
                    t.act(DECT.v(), TMPD.v(), AF.Exp, bias=ngc, scale=1.0)
                    EGB = roles.get(f"EGB{hd}", 1)
                    t.act(EGB.v(), pg, AF.Exp)
                    yield
                    PK = ps.next()
                    pk = PK.v((slice(None), slice(0, 128)))
                    t.mm(pk, KT, KT)
                    PQ = ps.next()
                    pq = PQ.v((slice(None), slice(0, 128)))
                    t.mm(pq, KT, QT)
                    NT = roles.get(f"NT{hd}", 1)
                    t.tt(NT.v(), pk, DECT.v(), ALU.mult)
                    t.stt(NT.v(), NT.v(), beta, STRICT, ALU.mult, ALU.mult)
                    AQKT = roles.get(f"AQKT{hd}", 1)
                    t.stt(AQKT.v(), pq, QSCALE, DECT.v(), ALU.mult, ALU.mult)
                    yield
                    PM = ps.next()
                    pm = PM.v((slice(None), slice(0, 128)))
                    t.tp(pm, NT.v(), IDENT)
                    M = roles.get(f"M{hd}", 1)
                    t.cp(M.v(), pm, eng="act")
                    PT_ = roles.get(f"PT_{hd}", 1)
                    t.tt(PT_.v(), IDENT, NT.v(), ALU.subtract)
                    yield
                    Nk, Mk = NT, M
                    for lvl in range(6):
                        PA = ps.next()
                        pa = PA.v((slice(None), slice(0, 128)))
                        t.mm(pa, Mk.v(), Nk.v())
                        PB = ps.next()
                        pb = PB.v((slice(None), slice(0, 128)))
                        t.mm(pb, Nk.v(), Mk.v())
                        N2 = roles.get(f"N2{hd}", 2)
                        M2 = roles.get(f"M2{hd}", 2)
                        t.cp(N2.v(), pa, eng="act")
                        t.cp(M2.v(), pb)
                        yield
                        PC = ps.next()
                        pc = PC.v((slice(None), slice(0, 128)))
                        t.mm(pc, M2.v(), PT_.v())
                        PN_ = roles.get(f"PN_{hd}", 2)
                        t.tt(PN_.v(), pc, PT_.v(), ALU.add)
                        yield
                        PT_ = PN_
                        Nk, Mk = N2, M2
                    TTm = PT_
                    PKT = ps.next()
                    pkt = PKT.v((slice(None), slice(0, 128)))
                    t.tp(pkt, KT, IDENT)
                    XK = roles.get(f"XK{hd}", 1)
                    KD = roles.get(f"KD{hd}", 1)
                    t.act(XK.v(), pkt, AF.Copy, scale=egc)
                    t.act(KD.v(), pkt, AF.Copy, scale=EKD)
                    yield
                    PVT = ps.next()
                    pvt = PVT.v((slice(None), slice(0, 128)))
                    t.tp(pvt, VT, IDENT)
                    XV = roles.get(f"XV{hd}", 1)
                    t.cp(XV.v(), pvt, eng="act")
                    yield
                    PW = ps.next()
                    pw = PW.v((slice(None), slice(0, 128)))
                    t.mm(pw, XK.v(), TTm.v())
                    WT = roles.get(f"WT{hd}", 1)
                    t.cp(WT.v(), pw, eng="act")
                    yield
                    PU = ps.next()
                    pu = PU.v((slice(None), slice(0, 128)))
                    t.mm(pu, TTm.v(), XV.v())
                    UP = roles.get(f"UP{hd}", 1)
                    t.ts(UP.v(), pu, beta, None, ALU.mult)
                    yield
                    QDT = roles.get(f"QDT{hd}", 1)
                    t.stt(QDT.v(), QT, QSCALE, EGB.v(), ALU.mult, ALU.mult)
                    P1 = ps.next()
                    p1 = P1.v((slice(None), slice(0, 128)))
                    t.mm(p1, WT.v(), S[hd].v())
                    VN = roles.get(f"VN{hd}", 1)
                    t.stt(VN.v(), p1, nbeta, UP.v(), ALU.mult, ALU.add)
                    yield
                    P2 = ps.next()
                    p2 = P2.v((slice(None), slice(0, 128)))
                    t.mm(p2, S[hd].v(), QDT.v(), start=True, stop=False)
                    t.mm(p2, VN.v(), AQKT.v(), start=False, stop=True)
                    P3 = ps.next()
                    p3 = P3.v((slice(None), slice(0, 128)))
                    t.mm(p3, KD.v(), VN.v())
                    t.stt(S[hd].v(), S[hd].v(), EGL, p3, ALU.mult, ALU.add)
                    OT = roles.get(f"OT{hd}", 1)
                    t.cp(OT.v(), p2, eng="act")
                    SQO = roles.get(f"SQO{hd}", 1)
                    t.act(SQO.v(), p2, AF.Square)
                    yield
                    PR = ps.next()
                    pr_ = PR.v((slice(None), slice(0, 128)))
                    t.mm(pr_, ONES128, SQO.v())
                    RS = roles.get(f"RS{hd}", 1)
                    t.act(RS.v(), pr_, AF.Ln, bias=eps6.v(), scale=1.0)
                    t.act(RS.v(), RS.v(), AF.Exp, scale=-0.5)
                    yield
                    t.stt(OT.v(), OT.v(), hp.v((slice(None), slice(6, 7))), RS.v(), ALU.mult, ALU.mult)
                    t.tt(outa[hd].v((slice(None), bs)), OT.v(), ZT[hd].v((slice(None), bs)), ALU.mult)
                svc = SV[bl]
                def swa_gen(h):
                    PS_ = ps.next()
                    L = Lr.next()
                    if nb > 0:
                        lo = 0
                    else:
                        lo = 128
                    n = 256 - lo
                    pv = PS_.v((slice(None), slice(0, n)))
                    t.mm(pv, SQT[h].v((slice(None), bs)),
                         SKT.v((slice(None), slice(bl * 128 + lo, bl * 128 + 256))))
                    lv = L.v((slice(None), slice(0, n)))
                    t.stt(lv, pv, QSCALE, alibi.v((slice(None), h, slice(lo, 256))), ALU.mult, ALU.add)
                    yield
                    mx = sc(22 + h)
                    nm = sc(24 + h)
                    t.rmax(mx, lv)
                    t.tt(mx, mx, hp.v((slice(None), slice(4 + h, 5 + h))), ALU.max)
                    t.ts(nm, mx, -1.0, None, ALU.mult)
                    rs = sc(22 + h)
                    es_ = sc(26 + h)
                    t.act(lv, lv, AF.Exp, bias=nm, scale=1.0)
                    t.op("dve", (lambda o, i: (lambda e: e.tensor_reduce(o, i, AX.X, ALU.add)))(rs.ap, lv.ap),
                         reads=[lv], writes=[rs])
                    t.act(es_, hp.v((slice(None), slice(4 + h, 5 + h))), AF.Exp, bias=nm, scale=1.0)
                    t.tt(rs, rs, es_, ALU.add)
                    t.op("dve", (lambda o, i: (lambda e: e.reciprocal(o, i)))(rs.ap, rs.ap), reads=[rs], writes=[rs])
                    t.ts(lv, lv, rs, None, ALU.mult)
                    yield
                    pts = []
                    for hf in range(n // 128):
                        PTP = ps.next()
                        ptp = PTP.v((slice(None), slice(0, 128)))
                        t.tp(ptp, L.v((slice(None), slice(hf * 128, (hf + 1) * 128))), IDENT)
                        pt = PTr.next()
                        t.cp(pt.v(), ptp, eng="act")
                        pts.append(pt)
                        yield
                    PO = ps.next()
                    po = PO.v((slice(None), slice(0, 128)))
                    if nb > 0:
                        t.mm(po, SVprev.v(), pts[0].v(), start=True, stop=False)
                        t.mm(po, svc.v(), pts[1].v(), start=False, stop=True)
                    else:
                        t.mm(po, svc.v(), pts[0].v(), start=True, stop=True)
                    t.cp(outb[h].v((slice(None), bs)), po, eng="act")
                gens = ([dn_gen(0), dn_gen(1)] if stage >= 3 else []) + [swa_gen(0), swa_gen(1)]
                while gens:
                    for gq in list(gens):
                        try:
                            next(gq)
                        except StopIteration:
                            gens.remove(gq)
                SVprev = svc
            t.cp(SKT.v((slice(None), slice(0, 128))), SKT.v((slice(None), slice(TT, TT + 128))))
            for h in range(2):
                t.dma("sp", V(outT.t[h * 128:(h + 1) * 128, t0:t0 + TT], outT.res), outa[h].v(), outa[h].v())
                t.dma("sp", V(outT.t[256 + h * 128:256 + (h + 1) * 128, t0:t0 + TT], outT.res), outb[h].v(), outb[h].v())
        t.emit()
    return nc


def _consts():
    i = np.arange(128)
    ident = np.eye(128, dtype=np.float32)
    ones = np.ones((128, 128), np.float32)
    ones128 = np.full((128, 128), 1.0 / 128, np.float32)
    tri = (i[:, None] <= i[None, :]).astype(np.float32)
    negm = np.where(i[:, None] <= i[None, :], 0.0, NEG).astype(np.float32)
    strict = (i[:, None] < i[None, :]).astype(np.float32)
    return np.ascontiguousarray(np.stack([ident, ones, ones128, tri, negm, strict], axis=1))


def _alibi(core):
    q = np.arange(128)[:, None]
    k = np.arange(256)[None, :]
    dist = q - k + 128
    valid = (dist >= 0) & (dist < 128)
    out = np.zeros((128, 2, 256), np.float32)
    for h in range(2):
        hq = 2 * core + h
        slope = np.float32(2.0) ** np.float32(-8.0 * (hq + 1) / 16)
        out[:, h, :] = np.where(valid, -slope * dist.astype(np.float32), np.float32(NEG))
    return out


def _pp(vec):
    return np.ascontiguousarray(vec.reshape(KC, 128).T)


_NC_CACHE = {}


def _get(kind, T):
    key = (kind, T)
    if key not in _NC_CACHE:
        if kind == "A":
            _NC_CACHE[key] = build_A(T)
        else:
            _NC_CACHE[key] = build_B(T, kind == "Bm")
    return _NC_CACHE[key]


def a_inputs(xT, w_in, conv_w, a_log, dt_bias, dn_norm_w, sinks, core):
    c = core
    cols = []
    for base in (0, 2048, 4096):
        for h in (2 * c, 2 * c + 1):
            cols.append(np.arange(base + h * 128, base + (h + 1) * 128))
    for h in (2 * c, 2 * c + 1):
        cols.append(np.arange(6144 + h * 128, 6144 + (h + 1) * 128))
    sq0 = 6144 + 2048 + 32
    for h in (2 * c, 2 * c + 1):
        cols.append(np.arange(sq0 + h * 128, sq0 + (h + 1) * 128))
    sk0 = sq0 + 2048
    kv = c // 2
    cols.append(np.arange(sk0 + kv * 128, sk0 + (kv + 1) * 128))
    wfm = np.ascontiguousarray(w_in[:, np.concatenate(cols)])
    b0 = 6144 + 2048
    a0 = b0 + 16
    sv0 = sk0 + 512
    tcols = np.concatenate([np.arange(sv0 + kv * 128, sv0 + (kv + 1) * 128),
                            [b0 + 2 * c, b0 + 2 * c + 1, a0 + 2 * c, a0 + 2 * c + 1]])
    wtm = np.zeros((w_in.shape[0], NTM), np.float32)
    wtm[:, :132] = w_in[:, tcols]
    convw = np.zeros((128, 6, 4), np.float32)
    ci = 0
    for base in (0, 2048, 4096):
        for h in (2 * c, 2 * c + 1):
            convw[:, ci, :] = conv_w[:, base + h * 128:base + (h + 1) * 128].T
            ci += 1
    hp = np.zeros((128, 8), np.float32)
    hp[:, 0:2] = a_log[2 * c:2 * c + 2][None, :]
    hp[:, 2:4] = dt_bias[2 * c:2 * c + 2][None, :]
    hp[:, 4:6] = sinks[2 * c:2 * c + 2][None, :]
    hp[:, 6] = dn_norm_w
    return {"xT": xT, "wfm": wfm, "wtm": wtm, "convw": convw, "hp": hp, "cst": _consts(), "alibi": _alibi(c)}


def run_A(xT, w_in, conv_w, a_log, dt_bias, dn_norm_w, sinks):
    T = xT.shape[1]
    nc = _get("A", T)
    in_maps = [a_inputs(xT, w_in, conv_w, a_log, dt_bias, dn_norm_w, sinks, c) for c in range(NCORE)]
    res = run_bass_kernel_spmd(nc, in_maps, core_ids=list(range(NCORE)))
    mixT = np.empty((D_MODEL, T), np.float32)
    for c in range(NCORE):
        o = res.results[c]["outT"]
        mixT[2 * c * 128:(2 * c + 2) * 128] = o[0:256]
        mixT[2048 + 2 * c * 128:2048 + (2 * c + 2) * 128] = o[256:512]
    return mixT


NB = 8


def run_B(xT, mixT, w_o, g1, b1, g2, b2, ffn, moe):
    T = xT.shape[1]
    Tc = T // NB
    nc = _get("Bm" if moe else "Bd", Tc)
    lnp = np.ascontiguousarray(np.stack([_pp(g1), _pp(b1), _pp(g2), _pp(b2)], axis=1))
    ones = np.full((128, 128), 1.0 / D_MODEL, np.float32)
    ident = np.eye(128, dtype=np.float32)
    base = {"w_o": w_o, "lnp": lnp, "onesd": ones, "ident": ident}
    if moe:
        rw, ew1, ew3, ew2 = ffn
        base.update({"rw": np.ascontiguousarray(rw.reshape(KC, 128, 8).transpose(1, 0, 2)), "w1": ew1, "w3": ew3, "w2": ew2})
    else:
        fw1, fw3, fw2 = ffn
        base.update({"w1": np.ascontiguousarray(fw1.reshape(D_MODEL, 4, 2048).transpose(1, 0, 2)),
                     "w3": np.ascontiguousarray(fw3.reshape(D_MODEL, 4, 2048).transpose(1, 0, 2)),
                     "w2": fw2.reshape(4, 2048, D_MODEL)})
    in_maps = []
    for c in range(NB):
        m = dict(base)
        m["xT"] = np.ascontiguousarray(xT[:, c * Tc:(c + 1) * Tc])
        m["mixT"] = np.ascontiguousarray(mixT[:, c * Tc:(c + 1) * Tc])
        in_maps.append(m)
    res = run_bass_kernel_spmd(nc, in_maps, core_ids=list(range(NB)))
    return np.concatenate([res.results[c]["yT"] for c in range(NB)], axis=1)


def kernel(x, w_in, conv_w, a_log, dt_bias, dn_norm_w, sinks, w_o, ln1_g, ln1_b,
           ffn_w1, ffn_w3, ffn_w2, router_w, exp_w1, exp_w3, exp_w2, ln2_g, ln2_b):
    f = lambda a: np.asarray(a, dtype=np.float32)
    xT = np.ascontiguousarray(f(x)[0].T)
    for i in range(DEPTH):
        mixT = run_A(xT, f(w_in[i]), f(conv_w[i]), f(a_log[i]), f(dt_bias[i]), f(dn_norm_w[i]), f(sinks[i]))
        j = i // 2
        if i % 2 == 0:
            ffn = (f(ffn_w1[j]), f(ffn_w3[j]), f(ffn_w2[j]))
        else:
            ffn = (f(router_w[j]), f(exp_w1[j]), f(exp_w3[j]), f(exp_w2[j]))
        xT = run_B(xT, mixT, f(w_o[i]), f(ln1_g[i]), f(ln1_b[i]), f(ln2_g[i]), f(ln2_b[i]), ffn, i % 2 == 1)
    return np.ascontiguousarray(xT.T)[None].astype(np.float32)
```
